# Optimizing a Trainium2 kernel written in Bass

```python
import jax, jax.numpy as jnp
from jax import lax
import numpy as np

D_MODEL = 1024
BATCH = 16
SEQ = 2048
DEPTH = 4

N_MIXERS = 2
HEAD_DIM = 64
ROPE_THETA = 10000.0
NORM_EPS = 1e-6
Q_BLOCK = 128
NSA_Q_HEADS = D_MODEL // HEAD_DIM
NSA_KV_HEADS = 4
NSA_GROUP = NSA_Q_HEADS // NSA_KV_HEADS
CMP_BLOCK = 32
CMP_STRIDE = 16
CMP_HIDDEN = 4 * HEAD_DIM
SEL_BLOCK = 64
SEL_TOPK = 8
SEL_Q_CHUNK = 32
NSA_WINDOW = 512
NSA_IN = NSA_Q_HEADS * HEAD_DIM + 6 * NSA_KV_HEADS * HEAD_DIM + 3 * NSA_Q_HEADS
SWA_Q_HEADS = D_MODEL // HEAD_DIM
SWA_KV_HEADS = 2
SWA_GROUP = SWA_Q_HEADS // SWA_KV_HEADS
SWA_WINDOW = 128
SWA_IN = SWA_Q_HEADS * HEAD_DIM + 2 * SWA_KV_HEADS * HEAD_DIM
MLP_HIDDEN = 4 * D_MODEL

kernel_name = "hybrid_nsa_swa_sink_sqrelu_adaln"


def rms_norm(x, g):
    xf = x.astype(jnp.float32)
    y = xf * lax.rsqrt(jnp.mean(xf * xf, axis=-1, keepdims=True) + NORM_EPS)
    return (y * g.astype(jnp.float32)).astype(x.dtype)


def modulate(h, shift, scale):
    return h * (1 + scale[:, None, :]) + shift[:, None, :]


def rope_tables(positions):
    inv = 1.0 / (ROPE_THETA ** (jnp.arange(0, HEAD_DIM, 2, dtype=jnp.float32) / HEAD_DIM))
    ang = positions.astype(jnp.float32)[..., None] * inv
    return jnp.cos(ang)[:, :, None, :], jnp.sin(ang)[:, :, None, :]


def apply_rope(x, cos, sin):
    x1, x2 = jnp.split(x.astype(jnp.float32), 2, axis=-1)
    return jnp.concatenate([x1 * cos - x2 * sin, x2 * cos + x1 * sin], axis=-1).astype(x.dtype)


def banded_attention(q, k, v, window, sinks):
    B, S, Hkv, G, dh = q.shape
    span = window + Q_BLOCK
    pad = ((0, 0), (window, 0), (0, 0), (0, 0))
    kp = jnp.pad(k, pad)
    vp = jnp.pad(v, pad)
    scale = dh ** -0.5

    def block(bi):
        s0 = bi * Q_BLOCK
        qb = lax.dynamic_slice_in_dim(q, s0, Q_BLOCK, axis=1)
        kb = lax.dynamic_slice_in_dim(kp, s0, span, axis=1)
        vb = lax.dynamic_slice_in_dim(vp, s0, span, axis=1)
        s = jnp.einsum('bqhgd,bkhd->bhgqk', qb, kb).astype(jnp.float32) * scale
        t = s0 + jnp.arange(Q_BLOCK)
        j = s0 - window + jnp.arange(span)
        dist = t[:, None] - j[None, :]
        mask = (dist >= 0) & (dist < window) & (j[None, :] >= 0)
        s = jnp.where(mask, s, -jnp.inf)
        if sinks is None:
            p = jax.nn.softmax(s, axis=-1)
        else:
            sk = sinks.astype(jnp.float32)[None, :, :, None, None]
            m = jnp.maximum(jnp.max(s, axis=-1, keepdims=True), sk)
            e = jnp.exp(s - m)
            p = e / (jnp.sum(e, axis=-1, keepdims=True) + jnp.exp(sk - m))
        return jnp.einsum('bhgqk,bkhd->bqhgd', p.astype(v.dtype), vb)

    out = lax.map(block, jnp.arange(S // Q_BLOCK))
    return jnp.moveaxis(out, 0, 1).reshape(B, S, Hkv, G, dh)


def compress_tokens(x, pe, w1, b1, w2, b2):
    B, S, Hkv, dh = x.shape
    n_cmp = (S - CMP_BLOCK) // CMP_STRIDE + 1
    idx = np.arange(n_cmp)[:, None] * CMP_STRIDE + np.arange(CMP_BLOCK)[None, :]
    xb = x[:, idx] + pe[None, None, :, None, :]
    xb = jnp.moveaxis(xb, 3, 2).reshape(B, n_cmp, Hkv, CMP_BLOCK * dh)
    return jax.nn.gelu(xb @ w1 + b1) @ w2 + b2


def compressed_attention(q, kc, vc):
    B, S, Hkv, G, dh = q.shape
    n_cmp = kc.shape[1]
    s = jnp.einsum('bshgd,bnhd->bhgsn', q, kc).astype(jnp.float32) * dh ** -0.5
    blk_end = jnp.arange(n_cmp) * CMP_STRIDE + CMP_BLOCK - 1
    mask = blk_end[None, :] <= jnp.arange(S)[:, None]
    s = jnp.where(mask, s, -jnp.inf)
    m = jnp.max(s, axis=-1, keepdims=True)
    m = jnp.where(jnp.isfinite(m), m, 0.0)
    e = jnp.where(mask, jnp.exp(s - m), 0.0)
    den = jnp.sum(e, axis=-1, keepdims=True)
    p = e / jnp.where(den > 0, den, 1.0)
    o = jnp.einsum('bhgsn,bnhd->bshgd', p.astype(vc.dtype), vc)
    return o, p


def select_blocks(p_cmp, S):
    n_cmp = p_cmp.shape[-1]
    n_sel = S // SEL_BLOCK
    cs = np.arange(n_cmp)[:, None] * CMP_STRIDE
    ss = np.arange(n_sel)[None, :] * SEL_BLOCK
    overlap = np.clip(np.minimum(cs + CMP_BLOCK, ss + SEL_BLOCK) - np.maximum(cs, ss), 0, None)
    M = jnp.asarray((overlap / CMP_STRIDE).astype(np.float32))
    imp = jnp.einsum('bhgsn,nj->bhsj', p_cmp, M)
    cur = jnp.arange(S) // SEL_BLOCK
    jj = jnp.arange(n_sel)
    causal = jj[None, :] <= cur[:, None]
    forced = (jj[None, :] == 0) | (jj[None, :] == cur[:, None]) | (jj[None, :] == cur[:, None] - 1)
    imp = jnp.where(forced, jnp.inf, jnp.where(causal, imp, -jnp.inf))
    _, idx = lax.top_k(imp, min(SEL_TOPK, n_sel))
    return idx


def selected_block_attention(q, k, v, sel_idx):
    B, S, Hkv, G, dh = q.shape
    n_sel = S // SEL_BLOCK
    kb = k.reshape(B, n_sel, SEL_BLOCK, Hkv, dh).transpose(0, 3, 1, 2, 4)
    vb = v.reshape(B, n_sel, SEL_BLOCK, Hkv, dh).transpose(0, 3, 1, 2, 4)
    bi = jnp.arange(B)[:, None, None, None]
    hi = jnp.arange(Hkv)[None, :, None, None]
    scale = dh ** -0.5

    def chunk(ci):
        s0 = ci * SEL_Q_CHUNK
        qc = lax.dynamic_slice_in_dim(q, s0, SEL_Q_CHUNK, axis=1)
        ic = lax.dynamic_slice_in_dim(sel_idx, s0, SEL_Q_CHUNK, axis=2)
        kg = kb[bi, hi, ic]
        vg = vb[bi, hi, ic]
        s = jnp.einsum('bqhgd,bhqnld->bhgqnl', qc, kg).astype(jnp.float32) * scale
        t = s0 + jnp.arange(SEL_Q_CHUNK)
        pos = ic[..., None] * SEL_BLOCK + jnp.arange(SEL_BLOCK)
        mask = (pos <= t[None, None, :, None, None])[:, :, None]
        s = jnp.where(mask, s, -jnp.inf)
        n = ic.shape[-1]
        p = jax.nn.softmax(s.reshape(B, Hkv, G, SEL_Q_CHUNK, n * SEL_BLOCK), axis=-1).reshape(s.shape)
        return jnp.einsum('bhgqnl,bhqnld->bqhgd', p.astype(v.dtype), vg)

    out = lax.map(chunk, jnp.arange(S // SEL_Q_CHUNK))
    return jnp.moveaxis(out, 0, 1).reshape(B, S, Hkv, G, dh)


def nsa_mixer(h, w_in, w_out, cmp_pe, phi_w1, phi_b1, phi_w2, phi_b2, cos, sin):
    B, S, _ = h.shape
    qd = NSA_Q_HEADS * HEAD_DIM
    kd = NSA_KV_HEADS * HEAD_DIM
    proj = h @ w_in
    splits = [qd + i * kd for i in range(7)]
    q, kc, vc, ks, vs, kw, vw, g = jnp.split(proj, splits, axis=-1)
    q = apply_rope(q.reshape(B, S, NSA_Q_HEADS, HEAD_DIM), cos, sin)
    q = q.reshape(B, S, NSA_KV_HEADS, NSA_GROUP, HEAD_DIM)
    kv_shape = (B, S, NSA_KV_HEADS, HEAD_DIM)
    kc = apply_rope(kc.reshape(kv_shape), cos, sin)
    ks = apply_rope(ks.reshape(kv_shape), cos, sin)
    kw = apply_rope(kw.reshape(kv_shape), cos, sin)
    vc, vs, vw = vc.reshape(kv_shape), vs.reshape(kv_shape), vw.reshape(kv_shape)
    kcc = compress_tokens(kc, cmp_pe[0], phi_w1[0], phi_b1[0], phi_w2[0], phi_b2[0])
    vcc = compress_tokens(vc, cmp_pe[1], phi_w1[1], phi_b1[1], phi_w2[1], phi_b2[1])
    o_cmp, p_cmp = compressed_attention(q, kcc, vcc)
    sel_idx = select_blocks(p_cmp, S)
    o_sel = selected_block_attention(q, ks, vs, sel_idx)
    o_win = banded_attention(q, kw, vw, NSA_WINDOW, None)
    g = jax.nn.sigmoid(g.astype(jnp.float32)).astype(h.dtype)
    g = g.reshape(B, S, NSA_KV_HEADS, NSA_GROUP, 3)
    o = g[..., 0:1] * o_cmp + g[..., 1:2] * o_sel + g[..., 2:3] * o_win
    return o.reshape(B, S, qd) @ w_out


def swa_sink_mixer(h, w_in, w_out, sinks, cos, sin):
    B, S, _ = h.shape
    qd = SWA_Q_HEADS * HEAD_DIM
    kd = SWA_KV_HEADS * HEAD_DIM
    q, k, v = jnp.split(h @ w_in, [qd, qd + kd], axis=-1)
    q = apply_rope(q.reshape(B, S, SWA_Q_HEADS, HEAD_DIM), cos, sin)
    q = q.reshape(B, S, SWA_KV_HEADS, SWA_GROUP, HEAD_DIM)
    k = apply_rope(k.reshape(B, S, SWA_KV_HEADS, HEAD_DIM), cos, sin)
    v = v.reshape(B, S, SWA_KV_HEADS, HEAD_DIM)
    o = banded_attention(q, k, v, SWA_WINDOW, sinks.reshape(SWA_KV_HEADS, SWA_GROUP))
    return o.reshape(B, S, qd) @ w_out


def squared_relu_mlp(h, w_up, w_down):
    a = jax.nn.relu(h @ w_up)
    return (a * a) @ w_down


def setup_inputs(seed: int = 0) -> dict:
    key = jax.random.key(seed)
    ks = jax.random.split(key, 20)
    n_a = (DEPTH + N_MIXERS - 1) // N_MIXERS
    n_b = DEPTH // N_MIXERS
    nrm = jax.random.normal
    f32 = jnp.float32
    x = nrm(ks[0], (BATCH, SEQ, D_MODEL), f32)
    c = nrm(ks[1], (BATCH, D_MODEL), f32)
    offset = jax.random.randint(ks[2], (BATCH, 1), 0, 4096, dtype=jnp.int32)
    positions = (offset + jnp.arange(SEQ, dtype=jnp.int32)[None, :]).astype(jnp.int32)
    ada_w = nrm(ks[3], (DEPTH, D_MODEL, 6 * D_MODEL), f32) * (0.5 * D_MODEL ** -0.5)
    ada_b = nrm(ks[4], (DEPTH, 6 * D_MODEL), f32) * 0.01
    norm_g = 1.0 + 0.02 * nrm(ks[5], (DEPTH, 4, D_MODEL), f32)
    nsa_w_in = nrm(ks[6], (n_a, D_MODEL, NSA_IN), f32) * D_MODEL ** -0.5
    nsa_w_out = nrm(ks[7], (n_a, NSA_Q_HEADS * HEAD_DIM, D_MODEL), f32) * (NSA_Q_HEADS * HEAD_DIM) ** -0.5
    nsa_cmp_pe = 0.1 * nrm(ks[8], (n_a, 2, CMP_BLOCK, HEAD_DIM), f32)
    nsa_phi_w1 = nrm(ks[9], (n_a, 2, CMP_BLOCK * HEAD_DIM, CMP_HIDDEN), f32) * (CMP_BLOCK * HEAD_DIM) ** -0.5
    nsa_phi_b1 = 0.01 * nrm(ks[10], (n_a, 2, CMP_HIDDEN), f32)
    nsa_phi_w2 = nrm(ks[11], (n_a, 2, CMP_HIDDEN, HEAD_DIM), f32) * CMP_HIDDEN ** -0.5
    nsa_phi_b2 = 0.01 * nrm(ks[12], (n_a, 2, HEAD_DIM), f32)
    swa_w_in = nrm(ks[13], (n_b, D_MODEL, SWA_IN), f32) * D_MODEL ** -0.5
    swa_w_out = nrm(ks[14], (n_b, SWA_Q_HEADS * HEAD_DIM, D_MODEL), f32) * (SWA_Q_HEADS * HEAD_DIM) ** -0.5
    swa_sinks = nrm(ks[15], (n_b, SWA_Q_HEADS), f32)
    mlp_w_up = nrm(ks[16], (DEPTH, D_MODEL, MLP_HIDDEN), f32) * D_MODEL ** -0.5
    mlp_w_down = nrm(ks[17], (DEPTH, MLP_HIDDEN, D_MODEL), f32) * MLP_HIDDEN ** -0.5
    return {"x": x, "c": c, "positions": positions, "ada_w": ada_w, "ada_b": ada_b,
            "norm_g": norm_g, "nsa_w_in": nsa_w_in, "nsa_w_out": nsa_w_out,
            "nsa_cmp_pe": nsa_cmp_pe, "nsa_phi_w1": nsa_phi_w1, "nsa_phi_b1": nsa_phi_b1,
            "nsa_phi_w2": nsa_phi_w2, "nsa_phi_b2": nsa_phi_b2, "swa_w_in": swa_w_in,
            "swa_w_out": swa_w_out, "swa_sinks": swa_sinks, "mlp_w_up": mlp_w_up,
            "mlp_w_down": mlp_w_down}


def reference(x, c, positions, ada_w, ada_b, norm_g, nsa_w_in, nsa_w_out, nsa_cmp_pe,
              nsa_phi_w1, nsa_phi_b1, nsa_phi_w2, nsa_phi_b2, swa_w_in, swa_w_out,
              swa_sinks, mlp_w_up, mlp_w_down):
    cos, sin = rope_tables(positions)
    cond = jax.nn.silu(c)
    for i in range(DEPTH):
        mod = cond @ ada_w[i] + ada_b[i]
        sh1, sc1, g1, sh2, sc2, g2 = jnp.split(mod, 6, axis=-1)
        h = modulate(rms_norm(x, norm_g[i, 0]), sh1, sc1)
        a = i // N_MIXERS
        if i % N_MIXERS == 0:
            y = nsa_mixer(h, nsa_w_in[a], nsa_w_out[a], nsa_cmp_pe[a], nsa_phi_w1[a],
                          nsa_phi_b1[a], nsa_phi_w2[a], nsa_phi_b2[a], cos, sin)
        else:
            y = swa_sink_mixer(h, swa_w_in[a], swa_w_out[a], swa_sinks[a], cos, sin)
        x = x + (1 + g1)[:, None, :] * rms_norm(y, norm_g[i, 1])
        h = modulate(rms_norm(x, norm_g[i, 2]), sh2, sc2)
        y = squared_relu_mlp(h, mlp_w_up[i], mlp_w_down[i])
        x = x + (1 + g2)[:, None, :] * rms_norm(y, norm_g[i, 3])
    return x
```

```python
import contextlib
import numpy as np
import concourse.bass as bass
import concourse.mybir as mybir
from concourse.bass_utils import run_bass_kernel_spmd

F32 = mybir.dt.float32
BF16 = mybir.dt.bfloat16
I32 = mybir.dt.int32
AF = mybir.ActivationFunctionType
ALU = mybir.AluOpType
AX = mybir.AxisListType

ENG = ('pe', 'act', 'dve', 'pool', 'sp')
CENG = ('pe', 'act', 'dve', 'pool')
SEM_CH = 20000


class Op(object):
    __slots__ = ('eng', 'fn', 'waits', 'signal', 'sigval', 'pos', 'dma_sem', 'dma_val',
                 'ksnap', 'ndma')


class Prog(object):
    def __init__(self):
        self.ops = {e: [] for e in ENG}
        self.lastw = {}
        self.readers = {}
        self.known = {e: {} for e in ENG}
        self.dma_cnt = {}
        self.dma_sems = []

    def _src(self, op):
        if op.dma_sem is not None:
            return ('d', op.dma_sem), op.dma_val
        return op.eng, op.pos

    def alias(self, new_key, old_keys):
        rd = {}
        for ok in old_keys:
            cands = []
            w = self.lastw.get(ok)
            if w is not None:
                cands.append(w)
            r = self.readers.get(ok)
            if r:
                cands.extend(r.values())
            for op in cands:
                src, val = self._src(op)
                if src not in rd or self._src(rd[src])[1] < val:
                    rd[src] = op
        if rd:
            self.readers[new_key] = rd

    def add(self, eng, fn, reads=(), writes=(), psum=(), dma_sem=None, ndma=1):
        op = Op()
        op.eng = eng
        op.fn = fn
        op.signal = False
        op.sigval = None
        op.dma_sem = dma_sem
        op.ndma = ndma
        op.pos = len(self.ops[eng]) + 1
        if dma_sem is not None:
            if dma_sem not in self.dma_cnt:
                self.dma_cnt[dma_sem] = 0
                self.dma_sems.append(dma_sem)
            self.dma_cnt[dma_sem] += ndma
            op.dma_val = self.dma_cnt[dma_sem] * 16
        else:
            op.dma_val = None
        raw = []
        other = []
        for k in reads:
            w = self.lastw.get(k)
            if w is not None:
                raw.append(w)
        wkeys = list(writes) + [('PS', b) for b in psum]
        for k in wkeys:
            w = self.lastw.get(k)
            if w is not None:
                other.append(w)
            rd = self.readers.get(k)
            if rd:
                other.extend(rd.values())
        known = self.known[eng]
        waits = {}
        is_dma = dma_sem is not None

        def need(d, is_raw):
            if d.dma_sem is None and d.eng == eng and not is_dma and not is_raw:
                return
            src, val = self._src(d)
            if known.get(src, 0) >= val:
                return
            if waits.get(src, (0, None))[0] < val:
                waits[src] = (val, d)

        for d in raw:
            need(d, True)
        for d in other:
            need(d, False)
        op.waits = []
        for src, (val, d) in waits.items():
            known[src] = val
            d.signal = True
            op.waits.append(d)
            if d.ksnap is not None:
                for s2, v2 in d.ksnap:
                    if known.get(s2, 0) < v2:
                        known[s2] = v2
        if dma_sem is None:
            op.ksnap = tuple((e, known.get(e, 0)) for e in CENG if known.get(e, 0))
        else:
            op.ksnap = None
        for k in reads:
            rd = self.readers.get(k)
            if rd is None:
                rd = self.readers[k] = {}
            rd[self._src(op)[0]] = op
        for k in wkeys:
            self.lastw[k] = op
            self.readers[k] = {}
        self.ops[eng].append(op)
        return op

    def pe(self, fn, reads=(), writes=(), psum=()):
        return self.add('pe', fn, reads, writes, psum)

    def act(self, fn, reads=(), writes=(), psum=()):
        return self.add('act', fn, reads, writes, psum)

    def dve(self, fn, reads=(), writes=(), psum=()):
        return self.add('dve', fn, reads, writes, psum)

    def pool(self, fn, reads=(), writes=(), psum=()):
        return self.add('pool', fn, reads, writes, psum)

    def dma(self, q, sem, fn, reads=(), writes=(), ndma=1):
        return self.add(q, fn, reads, writes, (), dma_sem=sem, ndma=ndma)

    def emit(self, nc, final_waits=()):
        nsig = {}
        for e in ENG:
            c = 0
            for op in self.ops[e]:
                if op.dma_sem is None and op.signal:
                    c += 1
                    op.sigval = c
            nsig[e] = c
        with contextlib.ExitStack() as es:
            csem = {}
            for e in CENG:
                n = (nsig[e] + SEM_CH - 1) // SEM_CH
                csem[e] = [es.enter_context(nc.semaphore("s_%s_%d" % (e, i)))
                           for i in range(max(n, 1))]
            dsem = {}
            for i, s in enumerate(self.dma_sems):
                dsem[s] = es.enter_context(nc.semaphore("d%d" % i))
            block = es.enter_context(nc.Block())

            def semval(d):
                if d.dma_sem is not None:
                    return dsem[d.dma_sem], d.dma_val
                i = (d.sigval - 1) // SEM_CH
                return csem[d.eng][i], (d.sigval - 1) % SEM_CH + 1

            def run(e, eobj):
                for op in self.ops[e]:
                    for d in op.waits:
                        s, v = semval(d)
                        eobj.wait_ge(s, v)
                    r = op.fn(eobj)
                    if op.dma_sem is not None:
                        if not isinstance(r, (list, tuple)):
                            r = [r]
                        assert len(r) == op.ndma, (len(r), op.ndma)
                        for ins in r:
                            ins.then_inc(dsem[op.dma_sem], 16)
                    elif op.signal:
                        if isinstance(r, (list, tuple)):
                            r = r[-1]
                        s, v = semval(op)
                        r.then_inc(s, 1)
                if e == 'sp':
                    for d in final_waits:
                        s, v = semval(d)
                        eobj.wait_ge(s, v)

            @block.tensor
            def _(t):
                run('pe', t)

            @block.scalar
            def _(t):
                run('act', t)

            @block.vector
            def _(t):
                run('dve', t)

            @block.gpsimd
            def _(t):
                run('pool', t)

            @block.sync
            def _(t):
                run('sp', t)


class Buf(object):
    def __init__(self, P, name, start, words, ghosts):
        self.P = P
        self.name = name
        self.start = start
        self.words = words
        self.ghosts = ghosts
        self.keys = []
        self.keyset = set()

    def k(self, *idx):
        key = (self.name,) + idx
        if key not in self.keyset:
            self.keyset.add(key)
            self.keys.append(key)
            if self.ghosts:
                self.P.alias(key, self.ghosts)
        return key


class Arena(object):
    def __init__(self, P, tensor, words):
        self.P = P
        self.t = tensor
        self.words = words
        self.top = 0
        self.live = []
        self.ghosts = []
        self.n = 0
        self.peak = 0

    def alloc(self, name, words):
        words = (words + 3) // 4 * 4
        s, e = self.top, self.top + words
        assert e <= self.words, "SBUF arena overflow: %s needs %d, top %d of %d" % (
            name, words, self.top, self.words)
        self.top = e
        self.peak = max(self.peak, e)
        gk = []
        for (gs, ge, keys) in self.ghosts:
            if gs < e and ge > s:
                gk.extend(keys)
        self.n += 1
        b = Buf(self.P, "%s#%d" % (name, self.n), s, words, gk)
        self.live.append(b)
        return b

    def mark(self):
        return (self.top, len(self.live))

    def release(self, mark):
        top, nlive = mark
        for b in self.live[nlive:]:
            self.ghosts = [g for g in self.ghosts if not (g[0] >= b.start and g[1] <= b.start + b.words)]
            self.ghosts.append((b.start, b.start + b.words, list(b.keys)))
        del self.live[nlive:]
        self.top = top

    def f32(self, b, n=None, off=0):
        n = b.words - off if n is None else n
        return self.t[:, b.start + off:b.start + off + n]

    def bf(self, b, n=None, off=0):
        n = 2 * b.words - off if n is None else n
        assert off % 2 == 0
        w0 = b.start + off // 2
        return self.t[:, w0:w0 + (n + 1) // 2].bitcast(BF16)[:, 0:n]

    def i32(self, b, n=None, off=0):
        n = b.words - off if n is None else n
        return self.t[:, b.start + off:b.start + off + n].bitcast(I32)


class Ring(object):
    def __init__(self, items):
        self.items = items
        self.i = 0

    def next(self):
        it = self.items[self.i % len(self.items)]
        self.i += 1
        return it


D = 1024
S = 2048
NT = 16
HID = 4096
DEPTH = 4
BIG = 30000.0
BIGV = 1.0e9
EPS = 1e-6
ARENA_WORDS = 53000
PI = float(np.pi)


def host_constants():
    c = {}
    ident = np.eye(128, dtype=np.float32)
    rot = np.zeros((128, 128), np.float32)
    for m in range(128):
        if (m % 64) < 32:
            rot[m + 32, m] = -1.0
        else:
            rot[m - 32, m] = 1.0
    k = np.arange(128)[:, None]
    q = np.arange(128)[None, :]
    triD = np.where(k <= q, 0.0, -BIG).astype(np.float32)
    triW = np.where(k > q, 0.0, -BIG).astype(np.float32)
    c['cbf'] = np.concatenate([ident, rot, np.tile(triD, (1, 4)), np.tile(triW, (1, 4))], axis=1)
    E = np.zeros((128, 2048), np.float32)
    for j in range(32):
        E[j, j * 64:(j + 1) * 64] = BIG
    c['E'] = E
    cs = np.arange(127)[:, None] * 16
    ss = np.arange(32)[None, :] * 64
    ov = np.clip(np.minimum(cs + 32, ss + 64) - np.maximum(cs, ss), 0, None)
    M = np.zeros((128, 32), np.float32)
    M[:127] = ov / 16.0
    r = np.arange(128)[:, None]
    m = np.arange(248)[None, :]
    cm = np.where((m - 120) <= np.floor((r - 31) / 16.0), 0.0, -BIG).astype(np.float32)
    c['cbf2'] = np.concatenate([M, cm], axis=1)
    A = np.zeros((128, 16, 32), np.float32)
    B = np.zeros((128, 16, 32), np.float32)
    for qi in range(16):
        t = 128 * qi + np.arange(128)[:, None]
        cur = t // 64
        j = np.arange(32)[None, :]
        forced = (j == 0) | (j == cur) | (j == cur - 1)
        causal = j <= cur
        A[:, qi, :] = (causal & ~forced).astype(np.float32)
        B[:, qi, :] = np.where(forced, BIGV, np.where(causal, 0.0, -BIGV))
    inv = (1.0 / (10000.0 ** (np.arange(0, 64, 2, dtype=np.float32) / 64))).astype(np.float32)
    invcol = np.tile(inv, 4).reshape(128, 1)
    c['cf32'] = np.concatenate([A.reshape(128, 512), B.reshape(128, 512), invcol,
                                np.zeros((128, 3), np.float32)], axis=1).astype(np.float32)
    return c


def permute_weights(nsa_w_in, swa_w_in):
    cols = []
    for t in range(8):
        if t < 4:
            lo, hi = t, t + 4
        else:
            lo, hi = 8 + (t - 4), 12 + (t - 4)
        cols += list(range(lo * 64, lo * 64 + 64)) + list(range(hi * 64, hi * 64 + 64))
    cols += list(range(1024, 1280))
    cols += list(range(1280, 1536))
    cols += list(range(1536, 1792))
    cols += list(range(2048, 2304))
    nsa_fm = np.ascontiguousarray(nsa_w_in[:, :, cols])
    tm = list(range(1792, 2048)) + list(range(2304, 2560)) + list(range(2560, 2608))
    nsa_tm = np.ascontiguousarray(nsa_w_in[:, :, tm])
    cols = []
    for t in range(8):
        cols += list(range(t * 64, t * 64 + 64)) + list(range((t + 8) * 64, (t + 8) * 64 + 64))
    cols += list(range(1024, 1152))
    swa_fm = np.ascontiguousarray(swa_w_in[:, :, cols])
    swa_tm = np.ascontiguousarray(swa_w_in[:, :, 1152:1280])
    return nsa_fm, nsa_tm, swa_fm, swa_tm


def build_program(layers=None, nseq=2, debug=None):
    if layers is None:
        layers = [(l, ('mix', 'mlp')) for l in range(DEPTH)]
    nc = bass.Bass("TRN2", target_bir_lowering=False)
    es = contextlib.ExitStack()

    def din(name, shape, dt=F32):
        return nc.dram_tensor(name, list(shape), dt, kind="ExternalInput").ap()

    x_d = din("x", [2, S, D])
    c_d = din("c", [2, D])
    pos_d = din("pos", [2, S], I32)
    ada_w_d = din("ada_w", [4, D, 6 * D])
    ada_b_d = din("ada_b", [4, 6 * D])
    norm_g_d = din("norm_g", [4, 4, D])
    nsa_fm_d = din("nsa_fm", [2, D, 2048])
    nsa_tm_d = din("nsa_tm", [2, D, 560])
    nsa_wo_d = din("nsa_wo", [2, D, D])
    pe_d = din("cmp_pe", [2, 2, 32, 64])
    w1_d = din("phi_w1", [2, 2, 2048, 256])
    b1_d = din("phi_b1", [2, 2, 256])
    w2_d = din("phi_w2", [2, 2, 256, 64])
    b2_d = din("phi_b2", [2, 2, 64])
    swa_fm_d = din("swa_fm", [2, D, 1152])
    swa_tm_d = din("swa_tm", [2, D, 128])
    swa_wo_d = din("swa_wo", [2, D, D])
    sinks_d = din("swa_sinks", [2, 16])
    wup_d = din("mlp_w_up", [4, D, HID])
    wdn_d = din("mlp_w_down", [4, HID, D])
    cbf_d = din("cbf", [128, 1280])
    E_d = din("cE", [128, 2048])
    cbf2_d = din("cbf2", [128, 280])
    cf32_d = din("cf32", [128, 1028])
    out_d = nc.dram_tensor("out", [2, S, D], F32, kind="ExternalOutput").ap()
    modv_d = nc.dram_tensor("modv", [4, 2, 6, D], F32).ap()
    q_d = nc.dram_tensor("qscr", [128, 8, S], BF16).ap()

    arena_t = es.enter_context(nc.sbuf_tensor("arena", [128, ARENA_WORDS], F32))
    ps = [es.enter_context(nc.psum_tensor("ps%d" % i, [128, 512], F32)) for i in range(8)]
    P = Prog()
    A = Arena(P, arena_t, ARENA_WORDS)
    MUL, ADD, SUB, MAX = ALU.mult, ALU.add, ALU.subtract, ALU.max

    def psb(b):
        return ps[b][:, :].bitcast(BF16)

    CBb = A.alloc("cbf", 640)
    cbf = A.bf(CBb)
    ident = cbf[:, 0:128]
    rot = cbf[:, 128:256]
    triD = cbf[:, 256:768]
    triW = cbf[:, 768:1280]
    Eb = A.alloc("E", 1024)
    Emat = A.bf(Eb)
    CB2b = A.alloc("cbf2", 140)
    cbf2 = A.bf(CB2b)
    Mmat = cbf2[:, 0:32]
    cmask = cbf2[:, 32:280]
    CFb = A.alloc("cf32", 1028)
    cf32 = A.f32(CFb)
    Aadj = cf32[:, 0:512].rearrange("p (q j) -> p q j", q=16)
    Badj = cf32[:, 512:1024].rearrange("p (q j) -> p q j", q=16)
    invc = cf32[:, 1024:1025]
    NEGPIb = A.alloc("negpi", 4)
    negpi = A.f32(NEGPIb, 1, 0)
    epsc = A.f32(NEGPIb, 1, 1)
    halfpi = A.f32(NEGPIb, 1, 2)

    P.dma('pool', 'c0', lambda e: e.dma_start(out=cbf, in_=cbf_d), writes=[CBb.k()])
    P.dma('pool', 'c1', lambda e: e.dma_start(out=Emat, in_=E_d), writes=[Eb.k()])
    P.dma('pool', 'c2', lambda e: e.dma_start(out=cbf2, in_=cbf2_d), writes=[CB2b.k()])
    P.dma('sp', 'c3', lambda e: e.dma_start(out=cf32, in_=cf32_d), writes=[CFb.k()])
    P.pool(lambda e: e.memset(negpi, -PI), writes=[NEGPIb.k(0)])
    P.pool(lambda e: e.memset(epsc, EPS), writes=[NEGPIb.k(1)])
    P.pool(lambda e: e.memset(halfpi, PI / 2), writes=[NEGPIb.k(2)])
    KC = [CBb.k(), Eb.k(), CB2b.k(), CFb.k(), NEGPIb.k(0), NEGPIb.k(1)]
    kIDENT = CBb.k()

    def phase0():
        mk = A.mark()
        cTb = A.alloc("cT", 16)
        cT = A.f32(cTb).rearrange("p (k b) -> p k b", k=8)
        condb = A.alloc("cond", 8)
        cond = A.bf(condb).rearrange("p (k b) -> p k b", k=8)
        tmpb = A.alloc("ctmp", 16)
        ctmp = A.f32(tmpb).rearrange("p (k b) -> p k b", k=8)
        P.dma('sp', 'p0a', lambda e: [e.dma_start(out=cT[:, :, b_], in_=c_d[b_].rearrange("(k p) -> p k", p=128))
                                      for b_ in range(2)], writes=[cTb.k()], ndma=2)
        P.act(lambda e: e.activation(out=ctmp, in_=cT, func=AF.Exp, scale=-1.0), reads=[cTb.k()], writes=[tmpb.k()])
        P.dve(lambda e: e.tensor_scalar(out=ctmp, in0=ctmp, scalar1=1.0, scalar2=None, op0=ADD),
              reads=[tmpb.k()], writes=[tmpb.k()])
        P.dve(lambda e: e.reciprocal(out=ctmp, in_=ctmp), reads=[tmpb.k()], writes=[tmpb.k()])
        P.dve(lambda e: e.tensor_tensor(out=cond, in0=ctmp, in1=cT, op=MUL), reads=[tmpb.k(), cTb.k()],
              writes=[condb.k()])
        NQ = 1536
        wbufs = [A.alloc("adaw", 8 * NQ // 2) for _ in range(2)]
        wring = Ring(list(range(2)))
        modb = A.alloc("modsb", 6 * D)
        modsb = A.f32(modb)
        abb = A.alloc("adab", 6 * D)
        adab = A.f32(abb)
        gb = A.alloc("g4", 4 * D)
        g4 = A.f32(gb).rearrange("p (a d) -> p a d", a=4)
        effb = A.alloc("eff", 6 * D)
        eff = A.f32(effb).rearrange("p (a d) -> p a d", a=6)
        bank = Ring([0, 1, 2, 3])
        for l in range(DEPTH):
            P.dma('sp', 'p0b', lambda e, l=l: e.dma_start(out=adab[0:2, :], in_=ada_b_d[l:l + 1, :].to_broadcast([2, 6 * D])),
                  writes=[abb.k()])
            P.dma('sp', 'p0g', lambda e, l=l: e.dma_start(
                out=g4[0:2], in_=norm_g_d[l:l + 1].to_broadcast([2, 4, D])), writes=[gb.k()])
            for qt in range(4):
                wi = wring.next()
                wb = wbufs[wi]
                wv = A.bf(wb).rearrange("p (k n) -> p k n", k=8)
                P.dma('pool', ('adaw', wi), lambda e, l=l, qt=qt, wv=wv: e.dma_start(
                    out=wv, in_=ada_w_d[l, :, qt * NQ:(qt + 1) * NQ].rearrange("(k p) n -> p k n", p=128)),
                    writes=[wb.k()])
                for nchk in range(3):
                    b = bank.next()
                    for k in range(8):
                        P.pe(lambda e, b=b, k=k, wv=wv, nchk=nchk: e.matmul(
                            ps[b][0:2, :], lhsT=cond[:, k, :], rhs=wv[:, k, nchk * 512:(nchk + 1) * 512],
                            start=(k == 0), stop=(k == 7)), reads=[condb.k(), wb.k()], psum=[b])
                    c0 = qt * NQ + nchk * 512
                    P.dve(lambda e, b=b, c0=c0: e.tensor_tensor(out=modsb[0:2, c0:c0 + 512], in0=ps[b][0:2, :],
                                                                 in1=adab[0:2, c0:c0 + 512], op=ADD),
                          reads=[abb.k()], writes=[modb.k(c0)], psum=[b])
            allmod = [modb.k(c0) for c0 in range(0, 6 * D, 512)]
            plan = [(0, 1, 0), (2, 2, 1), (3, 4, 2), (5, 5, 3)]
            for (ei, mi, gi) in plan:
                P.dve(lambda e, ei=ei, mi=mi, gi=gi: e.scalar_tensor_tensor(
                    out=eff[0:2, ei, :], in0=modsb[0:2, mi * D:(mi + 1) * D], scalar=1.0, in1=g4[0:2, gi, :],
                    op0=ADD, op1=MUL), reads=allmod + [gb.k()], writes=[effb.k(ei)])
            for (ei, mi) in [(1, 0), (4, 3)]:
                P.dve(lambda e, ei=ei, mi=mi: e.tensor_copy(out=eff[0:2, ei, :], in_=modsb[0:2, mi * D:(mi + 1) * D]),
                      reads=allmod, writes=[effb.k(ei)])
            P.dma('sp', 'p0o', lambda e, l=l: e.dma_start(out=modv_d[l], in_=eff[0:2]),
                  reads=[effb.k(i) for i in range(6)], writes=[('modv_d', l)])
        A.release(mk)

    def load_mod(l, s, first):
        P.dma('sp', 'modl', lambda e: e.dma_start(
            out=MOD, in_=modv_d[l, s:s + 1, first:first + 3, :].to_broadcast([128, 3, D])),
            reads=[('modv_d', l)], writes=[MODb.k()])

    def rstd_from_ss(ss_ap, ss_key, n, out_ap, out_key):
        P.act(lambda e: e.activation(out=out_ap, in_=ss_ap, func=AF.Ln, scale=1.0 / D, bias=epsc),
              reads=[ss_key, NEGPIb.k(1)], writes=[out_key])
        P.act(lambda e: e.activation(out=out_ap, in_=out_ap, func=AF.Exp, scale=-0.5),
              reads=[out_key], writes=[out_key])

    def prenorm_tiles(tiles, hT, hTb, col0, bank_ring, tmpring, use_pool=True):
        ssap, sskey = small_ring.next()
        n = len(tiles)
        for i, t in enumerate(tiles):
            P.act(lambda e, t=t, i=i: e.activation(out=junk, in_=X[:, t, :], func=AF.Square,
                                                   accum_out=ssap[:, i:i + 1]),
                  reads=[Xb.k(t)], writes=[JKb.k(), sskey])
        rsap, rskey = small_ring.next()
        rstd_from_ss(ssap[:, 0:n], sskey, n, rsap[:, 0:n], rskey)
        for i, t in enumerate(tiles):
            (t1, t1k), (hb, hbk) = tmpring.next()
            P.dve(lambda e, t=t, i=i, t1=t1: e.scalar_tensor_tensor(
                out=t1, in0=X[:, t, :], scalar=rsap[:, i:i + 1], in1=MOD[:, 0, :], op0=MUL, op1=MUL),
                reads=[Xb.k(t), rskey, MODb.k()], writes=[t1k])
            (P.pool if use_pool else P.dve)(lambda e, t1=t1, hb=hb: e.tensor_tensor(out=hb, in0=t1, in1=MOD[:, 1, :], op=ADD),
                                            reads=[t1k, MODb.k()], writes=[hbk])
            b = bank_ring.next()
            pb = psb(b).rearrange("p (c n) -> p c n", c=8)
            for c in range(8):
                P.pe(lambda e, c=c, hb=hb, pb=pb: e.transpose(out=pb[:, c, :], in_=hb[:, c * 128:(c + 1) * 128],
                                                              identity=ident),
                     reads=[hbk, kIDENT], psum=[b])
            cc = col0 + i * 128
            P.act(lambda e, pb=pb, cc=cc: e.activation(out=hT[:, :, cc:cc + 128], in_=pb, func=AF.Copy),
                  writes=[hTb.k(cc // 128)], psum=[b])

    def post_residual(t, ybanks, eidx, tmpring, use_pool=True):
        ssap, sskey = small_ring.next()
        for h, b in enumerate(ybanks):
            P.act(lambda e, h=h, b=b: e.activation(out=junk[:, 0:512], in_=ps[b][:, :], func=AF.Square,
                                                   accum_out=ssap[:, h:h + 1]),
                  writes=[JKb.k(), sskey], psum=[b])
        P.dve(lambda e: e.tensor_tensor(out=ssap[:, 2:3], in0=ssap[:, 0:1], in1=ssap[:, 1:2], op=ADD),
              reads=[sskey], writes=[sskey])
        rsap, rskey = small_ring.next()
        rstd_from_ss(ssap[:, 2:3], sskey, 1, rsap[:, 0:1], rskey)
        for h, b in enumerate(ybanks):
            (t1, t1k) = tmpring.next()
            P.dve(lambda e, h=h, b=b, t1=t1: e.scalar_tensor_tensor(
                out=t1, in0=ps[b][:, :], scalar=rsap[:, 0:1], in1=MOD[:, eidx, h * 512:(h + 1) * 512],
                op0=MUL, op1=MUL), reads=[rskey, MODb.k()], writes=[t1k], psum=[b])
            (P.pool if use_pool else P.dve)(lambda e, h=h, t1=t1, t=t: e.tensor_tensor(
                out=X[:, t, h * 512:(h + 1) * 512], in0=X[:, t, h * 512:(h + 1) * 512], in1=t1, op=ADD),
                reads=[t1k, Xb.k(t)], writes=[Xb.k(t)])

    def mlp_sublayer(l, s):
        load_mod(l, s, 3)
        mk = A.mark()
        hTb = A.alloc("hTc", 8 * 512 // 2)
        hT = A.bf(hTb).rearrange("p (c n) -> p c n", c=8)
        aTb = A.alloc("aT", 32 * 512 // 2)
        aT = A.bf(aTb).rearrange("p (c n) -> p c n", c=32)
        NSL = 4
        ups = [A.alloc("wup", 8 * 256 // 2) for _ in range(NSL)]
        dns = [A.alloc("wdn", 2 * 1024 // 2) for _ in range(NSL)]
        pnring = alloc_pn_tmps()
        rs = [A.alloc("relu", 512) for _ in range(2)]
        rring = Ring([(A.f32(rs[i]), rs[i].k()) for i in range(2)])
        pts = [A.alloc("post_t1", 512) for _ in range(2)]
        ptring = Ring([(A.f32(pts[i]), pts[i].k()) for i in range(2)])
        upbank = Ring([0, 1, 2, 3])
        trbank = Ring([4, 5])
        items = []
        for tc in range(4):
            for sl in range(16):
                items.append(('u', tc, sl))
            for sl in range(16):
                items.append(('d', tc, sl))
        PF = 3
        loaded = [0]

        def issue_loads(upto):
            while loaded[0] < min(upto, len(items)):
                kind, tc, sl = items[loaded[0]]
                idx = loaded[0]
                loaded[0] += 1
                if kind == 'u':
                    ub = ups[(tc * 16 + sl) % NSL]
                    uv = A.bf(ub).rearrange("p (k n) -> p k n", k=8)
                    P.dma('pool', ('wup', (tc * 16 + sl) % NSL), lambda e, sl=sl, uv=uv: e.dma_start(
                        out=uv, in_=wup_d[l, :, sl * 256:(sl + 1) * 256].rearrange("(k p) n -> p k n", p=128)),
                        writes=[ub.k()])
                else:
                    db = dns[(tc * 16 + sl) % NSL]
                    dv = A.bf(db).rearrange("p (j n) -> p j n", j=2)
                    P.dma('pool', ('wdn', (tc * 16 + sl) % NSL), lambda e, sl=sl, dv=dv: e.dma_start(
                        out=dv, in_=wdn_d[l, sl * 256:(sl + 1) * 256, :].rearrange("(j p) n -> p j n", p=128)),
                        writes=[db.k()])

        pos = 0
        issue_loads(PF)
        prenorm_tiles([0, 1, 2, 3], hT, hTb, 0, trbank, pnring, use_pool=False)
        for tc in range(4):
            hkeys = [hTb.k(i) for i in range(4)]
            for sl in range(16):
                issue_loads(pos + 1 + PF)
                pos += 1
                ub = ups[(tc * 16 + sl) % NSL]
                uv = A.bf(ub).rearrange("p (k n) -> p k n", k=8)
                for j in range(2):
                    hc = sl * 2 + j
                    b = upbank.next()
                    for k in range(8):
                        P.pe(lambda e, b=b, k=k, uv=uv, j=j: e.matmul(
                            ps[b][:, :], lhsT=uv[:, k, j * 128:(j + 1) * 128], rhs=hT[:, k, :],
                            start=(k == 0), stop=(k == 7)), reads=hkeys + [ub.k()], psum=[b])
                    r, rk = rring.next()
                    if hc % 2 == 0:
                        P.act(lambda e, b=b, r=r: e.activation(out=r, in_=ps[b][:, :], func=AF.Relu),
                              writes=[rk], psum=[b])
                        P.dve(lambda e, r=r, hc=hc: e.tensor_tensor(out=aT[:, hc, :], in0=r, in1=r, op=MUL),
                              reads=[rk], writes=[aTb.k(hc)])
                    else:
                        P.dve(lambda e, b=b, r=r: e.tensor_scalar(out=r, in0=ps[b][:, :], scalar1=0.0, scalar2=None,
                                                                  op0=MAX), writes=[rk], psum=[b])
                        P.act(lambda e, r=r, hc=hc: e.activation(out=aT[:, hc, :], in_=r, func=AF.Square),
                              reads=[rk], writes=[aTb.k(hc)])
            if tc + 1 < 4:
                prenorm_tiles([4 * (tc + 1) + i for i in range(4)], hT, hTb, 0, trbank, pnring, use_pool=False)
            for sl in range(16):
                issue_loads(pos + 1 + PF)
                pos += 1
                db = dns[(tc * 16 + sl) % NSL]
                dv = A.bf(db).rearrange("p (j n) -> p j n", j=2)
                for tt in range(4):
                    for j in range(2):
                        hc = sl * 2 + j
                        for h in range(2):
                            b = tt * 2 + h
                            P.pe(lambda e, b=b, j=j, hc=hc, tt=tt, h=h, dv=dv: e.matmul(
                                ps[b][:, :], lhsT=aT[:, hc, tt * 128:(tt + 1) * 128],
                                rhs=dv[:, j, h * 512:(h + 1) * 512],
                                start=(hc == 0), stop=(hc == 31)), reads=[aTb.k(hc), db.k()], psum=[b])
            for tt in range(4):
                post_residual(4 * tc + tt, [tt * 2, tt * 2 + 1], 2, ptring, use_pool=False)
        A.release(mk)

    def rope_tables(s):
        mk = A.mark()
        pib = A.alloc("posi", S)
        posi = A.i32(pib)
        pfb = A.alloc("posf", S)
        posf = A.f32(pfb)
        C1 = 6.28125
        C2 = 2.0 * np.pi - 6.28125
        P.dma('sp', 'pos', lambda e: e.dma_start(out=posi, in_=pos_d[s:s + 1, :].to_broadcast([128, S])),
              writes=[pib.k()])
        P.dve(lambda e: e.tensor_copy(out=posf, in_=posi), reads=[pib.k()], writes=[pfb.k()])
        P.dve(lambda e: e.tensor_scalar(out=posf, in0=posf, scalar1=invc, scalar2=None, op0=MUL),
              reads=[pfb.k(), CFb.k()], writes=[pfb.k()])
        kS, kC = CSb.k(1), CSb.k(0)
        P.dve(lambda e: e.tensor_scalar(out=COS, in0=posf, scalar1=float(1.0 / (2 * np.pi)), scalar2=None, op0=MUL),
              reads=[pfb.k()], writes=[kC])
        P.dve(lambda e: e.tensor_copy(out=posi, in_=COS), reads=[kC, pib.k()], writes=[pib.k()])
        P.dve(lambda e: e.tensor_copy(out=COS, in_=posi), reads=[pib.k()], writes=[kC])
        P.dve(lambda e: e.scalar_tensor_tensor(out=SIN, in0=COS, scalar=-C1, in1=posf, op0=MUL, op1=ADD),
              reads=[kC, pfb.k()], writes=[kS])
        P.dve(lambda e: e.scalar_tensor_tensor(out=SIN, in0=COS, scalar=-C2, in1=SIN, op0=MUL, op1=ADD),
              reads=[kC, kS], writes=[kS])
        P.dve(lambda e: e.tensor_scalar(out=COS, in0=SIN, scalar1=PI, scalar2=-2 * PI, op0=ALU.is_gt, op1=MUL),
              reads=[kS], writes=[kC])
        P.dve(lambda e: e.tensor_tensor(out=SIN, in0=SIN, in1=COS, op=ADD), reads=[kS, kC], writes=[kS])
        P.dve(lambda e: e.tensor_scalar(out=COS, in0=SIN, scalar1=-PI, scalar2=2 * PI, op0=ALU.is_lt, op1=MUL),
              reads=[kS], writes=[kC])
        P.dve(lambda e: e.tensor_tensor(out=SIN, in0=SIN, in1=COS, op=ADD), reads=[kS, kC], writes=[kS])
        P.dve(lambda e: e.scalar_tensor_tensor(out=COS, in0=SIN, scalar=-1.0, in1=SIN, op0=MUL, op1=MAX), reads=[kS], writes=[kC])
        P.act(lambda e: e.activation(out=SIN, in_=SIN, func=AF.Sin), reads=[kS], writes=[kS])
        P.act(lambda e: e.activation(out=COS, in_=COS, func=AF.Sin, scale=-1.0, bias=halfpi),
              reads=[kC, NEGPIb.k(2)], writes=[kC])
        A.release(mk)

    def fm_project(hT, hkeys, wv, wkey, j, ntok0, tokbase, dest_fn, do_rope, banks, rtmp):
        b = banks.next()
        for k in range(8):
            P.pe(lambda e, b=b, k=k: e.matmul(ps[b][:, :], lhsT=wv[:, k, j * 128:(j + 1) * 128],
                                              rhs=hT[:, k, ntok0:ntok0 + 512], start=(k == 0), stop=(k == 7)),
                 reads=hkeys + [wkey], psum=[b])
        dst, dkey = dest_fn()
        if not do_rope:
            P.act(lambda e, b=b: e.activation(out=dst, in_=ps[b][:, :], func=AF.Copy), writes=[dkey], psum=[b])
            return
        (qraw, qrk), (t1, t1k), (t2, t2k) = rtmp.next()
        P.act(lambda e, b=b: e.activation(out=qraw, in_=ps[b][:, :], func=AF.Copy), writes=[qrk], psum=[b])
        b2 = banks.next()
        P.pe(lambda e, b2=b2: e.matmul(ps[b2][:, :], lhsT=rot, rhs=qraw, start=True, stop=True),
             reads=[qrk, kIDENT], psum=[b2])
        P.dve(lambda e, b=b: e.tensor_tensor(out=t1, in0=ps[b][:, :], in1=COS[:, tokbase:tokbase + 512], op=MUL),
              reads=[CSb.k(0)], writes=[t1k], psum=[b])
        P.dve(lambda e, b2=b2: e.tensor_tensor(out=t2, in0=ps[b2][:, :], in1=SIN[:, tokbase:tokbase + 512], op=MUL),
              reads=[CSb.k(1)], writes=[t2k], psum=[b2])
        P.pool(lambda e: e.tensor_tensor(out=dst, in0=t1, in1=t2, op=ADD), reads=[t1k, t2k], writes=[dkey])

    def alloc_rope_tmps(n=2):
        b_ = A.alloc("rt1", 512)
        c_ = A.alloc("rt2", 512)
        items = []
        for i in range(n):
            a = A.alloc("qraw", 256)
            items.append(((A.bf(a), a.k()), (A.f32(b_), b_.k()), (A.f32(c_), c_.k())))
        return Ring(items)

    def alloc_pn_tmps():
        t1 = A.alloc("pn_t1", D)
        hbs = [A.alloc("pn_hb", D // 2) for _ in range(2)]
        return Ring([((A.f32(t1), t1.k()), (A.bf(hbs[i]), hbs[i].k())) for i in range(2)])

    def attn_out_and_residual(t, oacc, oacck, obf, obfk, oT, oTb, wo, wokey, ptring, ybanks, trb):
        P.pool(lambda e: e.tensor_copy(out=obf, in_=oacc), reads=oacck, writes=[obfk])
        pb = psb(trb).rearrange("p (c n) -> p c n", c=8)
        for c in range(8):
            P.pe(lambda e, c=c: e.transpose(out=pb[:, c, :], in_=obf[:, c * 128:(c + 1) * 128], identity=ident),
                 reads=[obfk, kIDENT], psum=[trb])
        P.dve(lambda e: e.tensor_copy(out=oT, in_=pb), writes=[oTb.k()], psum=[trb])
        for h, b in enumerate(ybanks):
            for c in range(8):
                P.pe(lambda e, h=h, b=b, c=c: e.matmul(ps[b][:, :], lhsT=oT[:, c, :],
                                                       rhs=wo[:, c, h * 512:(h + 1) * 512],
                                                       start=(c == 0), stop=(c == 7)),
                     reads=[oTb.k(), wokey], psum=[b])
        post_residual(t, ybanks, 2, ptring)

    def alloc_q_slots():
        items = []
        for i in range(2):
            ba = A.alloc("qA", 8 * 128 // 2)
            bb = A.alloc("qB", 8 * 128 // 2)
            qa = A.bf(ba).rearrange("p (c n) -> p c n", c=8)
            qb_ = A.bf(bb).rearrange("p (c n) -> p c n", c=8)
            P.pool(lambda e, qa=qa: e.memset(qa[64:128], 0.0), writes=[ba.k('z')])
            P.pool(lambda e, qb_=qb_: e.memset(qb_[0:64], 0.0), writes=[bb.k('z')])
            items.append((i, ba, bb, qa, qb_))
        return Ring(items)

    def load_q_tile(ring, qi):
        i, ba, bb, qa, qb_ = ring.next()
        P.dma('sp', ('qt', i), lambda e: [
            e.dma_start(out=qa[0:64], in_=q_d[0:64, :, qi * 128:(qi + 1) * 128]),
            e.dma_start(out=qb_[64:128], in_=q_d[64:128, :, qi * 128:(qi + 1) * 128])],
            reads=[('q_d', ft, qi) for ft in range(8)], writes=[ba.k(), bb.k()], ndma=2)
        return (qa, qb_), [ba.k(), bb.k(), ba.k('z'), bb.k('z')]

    def run_jobs(jobs, emit_qk, emit_rest, hook=None):
        for jb in jobs[:2]:
            emit_qk(jb)
        for i, jb in enumerate(jobs):
            if i + 2 < len(jobs):
                emit_qk(jobs[i + 2])
            emit_rest(jb)
            if hook is not None and hook[1] is not None and i == min(hook[0], len(jobs) - 1):
                hook[1]()

    def swa_sublayer(l, s):
        a = l // 2
        load_mod(l, s, 0)
        mk = A.mark()
        kTb = A.alloc("kT", S // 2)
        kT = A.bf(kTb)
        Vb = A.alloc("V", NT * 2 * 65 // 2 + 2)
        V = A.bf(Vb, NT * 2 * 65).rearrange("p (t h d) -> p t h d", t=NT, h=2)
        esb = A.alloc("esink", 16)
        esink = A.f32(esb)
        P.dma('sp', 'sink', lambda e: e.dma_start(out=esink, in_=sinks_d[a:a + 1, :].to_broadcast([128, 16])),
              writes=[esb.k()])
        P.act(lambda e: e.activation(out=esink, in_=esink, func=AF.Exp), reads=[esb.k()], writes=[esb.k()])
        P.pool(lambda e: e.memset(V[:, :, :, 64:65], 1.0), writes=[Vb.k('ones')])
        mk2 = A.mark()
        hTb = A.alloc("hT", 8 * 512 // 2)
        hT = A.bf(hTb).rearrange("p (c n) -> p c n", c=8)
        pnring = alloc_pn_tmps()
        rtmp = alloc_rope_tmps(2)
        NSLOT = 4
        wbig = A.alloc("wbig", NSLOT * 1024)
        qst = [A.alloc("qst", 256) for _ in range(3)]
        qring = Ring([0, 1, 2])
        trbank = Ring([6, 7])
        pjbank = Ring([0, 1, 2, 3, 4, 5])
        items = [(ch, si) for ch in range(4) for si in range(6)]
        loaded = [0]
        PF = 3

        def slab(idx):
            return (A.bf(wbig, 8 * 256, (idx % NSLOT) * 8 * 256).rearrange("p (k n) -> p k n", k=8),
                    wbig.k('s', idx % NSLOT))

        def issue_loads(upto):
            while loaded[0] < min(upto, len(items)):
                idx = loaded[0]
                loaded[0] += 1
                ch, si = items[idx]
                wv, wk = slab(idx)
                if si < 4:
                    src, ncols = swa_fm_d[a, :, si * 256:(si + 1) * 256], 256
                elif si == 4:
                    src, ncols = swa_fm_d[a, :, 1024:1152], 128
                else:
                    src, ncols = swa_tm_d[a], 128
                P.dma('pool', ('wfm', idx % NSLOT), lambda e, wv=wv, src=src, ncols=ncols: e.dma_start(
                    out=wv[:, :, 0:ncols], in_=src.rearrange("(k p) n -> p k n", p=128)), writes=[wk])

        pos = 0
        issue_loads(PF)
        for ch in range(4):
            tiles = [4 * ch + i for i in range(4)]
            tok0 = ch * 512
            prenorm_tiles(tiles, hT, hTb, 0, trbank, pnring)
            hkeys = [hTb.k(i) for i in range(4)]
            for si in range(6):
                issue_loads(pos + 1 + PF)
                wv, wk = slab(pos)
                pos += 1
                if si < 4:
                    for j in range(2):
                        ft = si * 2 + j
                        qi_ = qring.next()
                        qb = qst[qi_]

                        def dest(qb=qb):
                            return A.bf(qb), qb.k()
                        fm_project(hT, hkeys, wv, wk, j, 0, tok0, dest, True, pjbank, rtmp)
                        P.dma('sp', ('qst', qi_), lambda e, qb=qb, ft=ft, tok0=tok0: e.dma_start(
                            out=q_d[:, ft, tok0:tok0 + 512], in_=A.bf(qb)), reads=[qb.k()],
                            writes=[('q_d', ft, tok0 // 128 + i) for i in range(4)])
                elif si == 4:
                    def dest(tok0=tok0):
                        return kT[:, tok0:tok0 + 512], kTb.k(tok0 // 512)
                    fm_project(hT, hkeys, wv, wk, 0, 0, tok0, dest, True, pjbank, rtmp)
                else:
                    for i, t in enumerate(tiles):
                        b = pjbank.next()
                        for k in range(8):
                            P.pe(lambda e, b=b, k=k, i=i, wv=wv: e.matmul(
                                ps[b][:, 0:128], lhsT=hT[:, k, i * 128:(i + 1) * 128], rhs=wv[:, k, 0:128],
                                start=(k == 0), stop=(k == 7)), reads=[hTb.k(i), wk], psum=[b])
                        P.dve(lambda e, b=b, t=t: e.tensor_copy(
                            out=V[:, t, :, 0:64], in_=ps[b][:, 0:128].rearrange("p (h d) -> p h d", h=2)),
                            writes=[Vb.k(t)], psum=[b])
        A.release(mk2)
        wob = A.alloc("wo", 8 * D // 2)
        wo = A.bf(wob).rearrange("p (k n) -> p k n", k=8)
        P.dma('pool', 'wo', lambda e: e.dma_start(out=wo, in_=swa_wo_d[a].rearrange("(k p) n -> p k n", p=128)),
              writes=[wob.k()])
        qring2 = alloc_q_slots()
        pts_ = [A.alloc("pT", 256) for _ in range(3)]
        ptr_ = Ring([0, 1, 2])
        oaccbs = [A.alloc("oacc", D) for _ in range(2)]
        prev_fin = None
        obfb = A.alloc("obf", D // 2)
        obf = A.bf(obfb)
        oTb = A.alloc("oT", D // 2)
        oT = A.bf(oTb).rearrange("p (c n) -> p c n", c=8)
        pts2 = [A.alloc("post_t1", 512) for _ in range(2)]
        ptring = Ring([(A.f32(pts2[i]), pts2[i].k()) for i in range(2)])
        sbank = Ring([3, 4, 7])
        obanks = {(0, 0): 0, (0, 1): 1, (1, 0): 0, (1, 1): 1}
        for qi in range(NT):
            qsel, qkeys = load_q_tile(qring2, qi)
            oaccb = oaccbs[qi % 2]
            oacc = A.f32(oaccb).rearrange("p (h d) -> p h d", h=16)
            kts = [kt for kt in (qi - 1, qi) if kt >= 0]
            jobs = []
            for kvh in range(2):
                for gh in range(2):
                    for ki, kt in enumerate(kts):
                        jobs.append(dict(kvh=kvh, gh=gh, ki=ki, kt=kt, nk=len(kts)))

            def emit_qk(jb, qi=qi, qsel=qsel, qkeys=qkeys):
                sb_ = sbank.next()
                jb['sb'] = sb_
                kt, gh, kvh = jb['kt'], jb['gh'], jb['kvh']
                P.pe(lambda e: e.matmul(
                    ps[sb_][:, :].rearrange("p (g n) -> p g n", g=4),
                    lhsT=kT[:, kt * 128:(kt + 1) * 128], rhs=qsel[kvh][:, 4 * gh:4 * gh + 4, :],
                    start=True, stop=False), reads=[kTb.k(kt // 4)] + qkeys, psum=[sb_])
                msk = triD if kt == qi else triW
                P.pe(lambda e: e.matmul(ps[sb_][:, :], lhsT=ident, rhs=msk, start=False, stop=True),
                     reads=[kIDENT], psum=[sb_])

            def emit_rest(jb, qi=qi, oacc=oacc, oaccb=oaccb):
                sb_, kt, gh, kvh, ki, nk = jb['sb'], jb['kt'], jb['gh'], jb['kvh'], jb['ki'], jb['nk']
                ob = obanks[(kvh, gh)]
                ov = ps[ob][:, 0:260].rearrange("p (g d) -> p g d", g=4)
                ptb = pts_[ptr_.next()]
                pt = A.bf(ptb)
                P.act(lambda e: e.activation(out=pt, in_=ps[sb_][:, :], func=AF.Exp, scale=0.125),
                      writes=[ptb.k()], psum=[sb_])
                for g in range(4):
                    P.pe(lambda e, g=g: e.matmul(
                        ov[:, g, :], lhsT=pt[:, g * 128:(g + 1) * 128], rhs=V[:, kt, kvh, :],
                        start=(ki == 0 and g == 0), stop=(ki == nk - 1), skip_group_check=True),
                        reads=[ptb.k(), Vb.k(kt), Vb.k('ones')], psum=[ob])
                if ki != nk - 1:
                    return
                h0 = kvh * 8 + gh * 4
                dn, dnk = small_ring.next()
                P.dve(lambda e: e.tensor_tensor(out=dn[:, 0:4], in0=ov[:, :, 64], in1=esink[:, h0:h0 + 4], op=ADD),
                      reads=[esb.k()], writes=[dnk], psum=[ob])
                P.dve(lambda e: e.reciprocal(out=dn[:, 4:8], in_=dn[:, 0:4]), reads=[dnk], writes=[dnk])
                P.dve(lambda e: e.tensor_tensor(
                    out=oacc[:, h0:h0 + 4, :], in0=ov[:, :, 0:64],
                    in1=dn[:, 4:8].unsqueeze(2).to_broadcast([128, 4, 64]), op=MUL),
                    reads=[dnk], writes=[oaccb.k(h0)], psum=[ob])

            def fin(qi=qi, oaccb=oaccb):
                attn_out_and_residual(qi, A.f32(oaccb), [oaccb.k(h0) for h0 in (0, 4, 8, 12)], obf, obfb.k(), oT, oTb,
                                      wo, wob.k(), ptring, [2, 5], 6)
            run_jobs(jobs, emit_qk, emit_rest, hook=(2, prev_fin))
            prev_fin = fin
        prev_fin()
        A.release(mk)

    def nsa_sublayer(l, s):
        a = l // 2
        load_mod(l, s, 0)
        mk = A.mark()
        ksTb = A.alloc("ksT", 2 * S // 2)
        ksT = A.bf(ksTb).rearrange("p (j n) -> p j n", j=2)
        kwTb = A.alloc("kwT", 2 * S // 2)
        kwT = A.bf(kwTb).rearrange("p (j n) -> p j n", j=2)
        VSb = A.alloc("VS", NT * 4 * 65 // 2 + 2)
        VS = A.bf(VSb, NT * 4 * 65).rearrange("p (t h d) -> p t h d", t=NT, h=4)
        VWb = A.alloc("VW", NT * 4 * 65 // 2 + 2)
        VW = A.bf(VWb, NT * 4 * 65).rearrange("p (t h d) -> p t h d", t=NT, h=4)
        GTb = A.alloc("gates", NT * 48)
        GT = A.f32(GTb).rearrange("p (t c) -> p t c", t=NT)
        kccb = A.alloc("kccT", 128)
        kccT = A.bf(kccb).rearrange("p (j n) -> p j n", j=2)
        vccb = A.alloc("vcc", 4 * 65 // 2 + 2)
        vcc = A.bf(vccb, 4 * 65).rearrange("p (h d) -> p h d", h=4)
        P.pool(lambda e: e.memset(VS[:, :, :, 64:65], 1.0), writes=[VSb.k('ones')])
        P.pool(lambda e: e.memset(VW[:, :, :, 64:65], 1.0), writes=[VWb.k('ones')])
        mkc = A.mark()
        kcTb = A.alloc("kcT", 2 * S // 2)
        kcT = A.bf(kcTb).rearrange("p (j n) -> p j n", j=2)
        vcTb = A.alloc("vcT", 2 * S // 2)
        vcT = A.bf(vcTb).rearrange("p (j n) -> p j n", j=2)
        mk2 = A.mark()
        hTb = A.alloc("hT", 8 * 512 // 2)
        hT = A.bf(hTb).rearrange("p (c n) -> p c n", c=8)
        pnring = alloc_pn_tmps()
        rtmp = alloc_rope_tmps(2)
        wbig = A.alloc("wbig", 4096)
        wring = Ring([0, 1])
        wtm = A.bf(wbig, 8 * 560, 0).rearrange("p (k n) -> p k n", k=8)
        wtmk = wbig.k('tm')
        qst = [A.alloc("qst", 256) for _ in range(3)]
        qring = Ring([0, 1, 2])
        gtmpb = A.alloc("gtmp", 48)
        gtmp = A.f32(gtmpb)
        trbank = Ring([6, 7])
        pjbank = Ring([0, 1, 2, 3, 4, 5])
        kdest = {8: (kcT, kcTb, 0), 9: (kcT, kcTb, 1), 10: (vcT, vcTb, 0), 11: (vcT, vcTb, 1),
                 12: (ksT, ksTb, 0), 13: (ksT, ksTb, 1), 14: (kwT, kwTb, 0), 15: (kwT, kwTb, 1)}
        for ch in range(4):
            tiles = [4 * ch + i for i in range(4)]
            tok0 = ch * 512
            prenorm_tiles(tiles, hT, hTb, 0, trbank, pnring)
            hkeys = [hTb.k(i) for i in range(4)]
            for sl in range(4):
                wi = wring.next()
                wv = A.bf(wbig, 8 * 512, wi * 8 * 512).rearrange("p (k n) -> p k n", k=8)
                wk = wbig.k('s', wi)
                P.dma('pool', ('wfm', wi), lambda e, sl=sl, wv=wv: e.dma_start(
                    out=wv, in_=nsa_fm_d[a, :, sl * 512:(sl + 1) * 512].rearrange("(k p) n -> p k n", p=128)),
                    writes=[wk, wtmk])
                for j in range(4):
                    ft = sl * 4 + j
                    if ft < 8:
                        qi_ = qring.next()
                        qb = qst[qi_]

                        def dest(qb=qb):
                            return A.bf(qb), qb.k()
                        fm_project(hT, hkeys, wv, wk, j, 0, tok0, dest, True, pjbank, rtmp)
                        P.dma('sp', ('qst', qi_), lambda e, qb=qb, ft=ft, tok0=tok0: e.dma_start(
                            out=q_d[:, ft, tok0:tok0 + 512], in_=A.bf(qb)), reads=[qb.k()],
                            writes=[('q_d', ft, tok0 // 128 + i) for i in range(4)])
                    else:
                        buf, bb, jj = kdest[ft]

                        def dest(buf=buf, bb=bb, jj=jj, tok0=tok0):
                            return buf[:, jj, tok0:tok0 + 512], bb.k(jj, tok0 // 512)
                        fm_project(hT, hkeys, wv, wk, j, 0, tok0, dest, ft not in (10, 11), pjbank, rtmp)
            P.dma('pool', 'wtm', lambda e: e.dma_start(out=wtm, in_=nsa_tm_d[a].rearrange("(k p) n -> p k n", p=128)),
                  writes=[wtmk, wbig.k('s', 0), wbig.k('s', 1)])
            for i, t in enumerate(tiles):
                b = pjbank.next()
                b2 = pjbank.next()
                for k in range(8):
                    P.pe(lambda e, b=b, k=k, i=i: e.matmul(ps[b][:, :], lhsT=hT[:, k, i * 128:(i + 1) * 128],
                                                           rhs=wtm[:, k, 0:512], start=(k == 0), stop=(k == 7)),
                         reads=[hTb.k(i), wtmk], psum=[b])
                for k in range(8):
                    P.pe(lambda e, b2=b2, k=k, i=i: e.matmul(ps[b2][:, 0:48], lhsT=hT[:, k, i * 128:(i + 1) * 128],
                                                             rhs=wtm[:, k, 512:560], start=(k == 0), stop=(k == 7)),
                         reads=[hTb.k(i), wtmk], psum=[b2])
                P.dve(lambda e, b=b, t=t: e.tensor_copy(
                    out=VS[:, t, :, 0:64], in_=ps[b][:, 0:256].rearrange("p (h d) -> p h d", h=4)),
                    writes=[VSb.k(t)], psum=[b])
                P.dve(lambda e, b=b, t=t: e.tensor_copy(
                    out=VW[:, t, :, 0:64], in_=ps[b][:, 256:512].rearrange("p (h d) -> p h d", h=4)),
                    writes=[VWb.k(t)], psum=[b])
                P.act(lambda e, b2=b2: e.activation(out=gtmp, in_=ps[b2][:, 0:48], func=AF.Exp, scale=-1.0),
                      writes=[gtmpb.k()], psum=[b2])
                P.pool(lambda e: e.tensor_scalar(out=gtmp, in0=gtmp, scalar1=1.0, scalar2=None, op0=ADD),
                       reads=[gtmpb.k()], writes=[gtmpb.k()])
                P.dve(lambda e, t=t: e.reciprocal(out=GT[:, t, :], in_=gtmp), reads=[gtmpb.k()], writes=[GTb.k(t)])
        A.release(mk2)
        mk3 = A.mark()
        w1b = A.alloc("w1", 32 * 256 // 2)
        w1 = A.bf(w1b).rearrange("p (l j) -> p l j", l=32)
        peb = A.alloc("peT", 16)
        peT = A.bf(peb)
        w2b = A.alloc("w2", 2 * 64 // 2)
        w2 = A.bf(w2b).rearrange("p (c d) -> p c d", c=2)
        b1b = A.alloc("b1T", 2)
        b1T = A.f32(b1b, 2)
        b2cb = A.alloc("b2c", 4)
        b2c = A.f32(b2cb, 1, 0)
        b2rb = A.alloc("b2r", 64)
        b2r = A.f32(b2rb)
        biasb = A.alloc("cbias", 2)
        cbias = A.f32(biasb, 2)
        gws = [A.alloc("gw%d" % i, 256) for i in range(4)]
        gx, gw_, ge, gr = [A.f32(g_).rearrange("p (c n) -> p c n", c=2) for g_ in gws]
        gTb = A.alloc("gT", 128)
        gT = A.bf(gTb).rearrange("p (c n) -> p c n", c=2)
        for kind in range(2):
            src = kcT if kind == 0 else vcT
            srcb = kcTb if kind == 0 else vcTb
            P.dma('pool', 'w1', lambda e, kind=kind: [
                e.dma_start(out=w1[0:64], in_=w1_d[a, kind].rearrange("(l d) j -> d l j", d=64)),
                e.dma_start(out=w1[64:128], in_=w1_d[a, kind].rearrange("(l d) j -> d l j", d=64))],
                writes=[w1b.k()], ndma=2)
            P.dma('pool', 'pe', lambda e, kind=kind: [
                e.dma_start(out=peT[0:64, :], in_=pe_d[a, kind].rearrange("l d -> d l")),
                e.dma_start(out=peT[64:128, :], in_=pe_d[a, kind].rearrange("l d -> d l"))],
                writes=[peb.k()], ndma=2)
            P.dma('pool', 'w2', lambda e, kind=kind: e.dma_start(
                out=w2, in_=w2_d[a, kind].rearrange("(c p) d -> p c d", p=128)), writes=[w2b.k()])
            P.dma('sp', 'b1', lambda e, kind=kind: e.dma_start(
                out=b1T, in_=b1_d[a, kind].rearrange("(c p) -> p c", p=128)), writes=[b1b.k()])
            if kind == 0:
                P.dma('sp', 'b2', lambda e: [
                    e.dma_start(out=b2c[0:64, :], in_=b2_d[a, 0].rearrange("(d o) -> d o", o=1)),
                    e.dma_start(out=b2c[64:128, :], in_=b2_d[a, 0].rearrange("(d o) -> d o", o=1))],
                    writes=[b2cb.k()], ndma=2)
            else:
                P.dma('sp', 'b2r', lambda e: e.dma_start(
                    out=b2r, in_=b2_d[a, 1:2, :].to_broadcast([128, 64])), writes=[b2rb.k()])
            for hk in range(4):
                j, half = hk // 2, hk % 2
                hs = slice(half * 64, half * 64 + 64)
                hb_ = 0
                hv = ps[hb_][:, 0:256].rearrange("p (c n) -> p c n", c=2)
                first = True
                for jc in range(2):
                    for li in range(32):
                        P.pe(lambda e, jc=jc, li=li, first=first, hs=hs, j=j, src=src: e.matmul(
                            hv[:, jc, 0:127], lhsT=w1[hs, li, jc * 128:(jc + 1) * 128],
                            rhs=src[hs, j, li:li + 16 * 126 + 1:16], start=first, stop=(li == 31),
                            skip_group_check=True),
                            reads=[w1b.k()] + [srcb.k(j, c_) for c_ in range(4)], psum=[hb_])
                        first = False
                        P.pe(lambda e, jc=jc, li=li, hs=hs: e.matmul(
                            hv[:, jc, 127:128], lhsT=w1[hs, li, jc * 128:(jc + 1) * 128], rhs=peT[hs, li:li + 1],
                            start=False, stop=(li == 31), skip_group_check=True),
                            reads=[w1b.k(), peb.k()], psum=[hb_])
                P.dve(lambda e, hv=hv: e.tensor_tensor(out=cbias, in0=hv[:, :, 127], in1=b1T, op=ADD),
                      reads=[b1b.k()], writes=[biasb.k()], psum=[hb_])
                for jc in range(2):
                    P.dve(lambda e, jc=jc, hv=hv: e.tensor_scalar(out=gx[:, jc, 0:127], in0=hv[:, jc, 0:127],
                                                                  scalar1=cbias[:, jc:jc + 1], scalar2=None, op0=ADD),
                          reads=[biasb.k()], writes=[gws[0].k(jc)], psum=[hb_])
                gxk = [gws[0].k(0), gws[0].k(1)]
                gx_, gwv, gev, grv = [g_[:, :, 0:127] for g_ in (gx, gw_, ge, gr)]
                P.pool(lambda e: e.tensor_tensor(out=gwv, in0=gx_, in1=gx_, op=MUL), reads=gxk, writes=[gws[1].k()])
                P.pool(lambda e: e.tensor_scalar(out=gwv, in0=gwv, scalar1=0.044715, scalar2=1.0, op0=MUL, op1=ADD),
                       reads=[gws[1].k()], writes=[gws[1].k()])
                P.pool(lambda e: e.tensor_tensor(out=gwv, in0=gwv, in1=gx_, op=MUL), reads=gxk + [gws[1].k()],
                       writes=[gws[1].k()])
                P.act(lambda e: e.activation(out=gev, in_=gwv, func=AF.Exp, scale=-1.5957691216057308),
                      reads=[gws[1].k()], writes=[gws[2].k()])
                P.pool(lambda e: e.tensor_scalar(out=gev, in0=gev, scalar1=1.0, scalar2=None, op0=ADD),
                       reads=[gws[2].k()], writes=[gws[2].k()])
                P.dve(lambda e: e.reciprocal(out=grv, in_=gev), reads=[gws[2].k()], writes=[gws[3].k()])
                P.pool(lambda e: e.tensor_tensor(out=gT[:, :, 0:127], in0=gx_, in1=grv, op=MUL),
                       reads=gxk + [gws[3].k()], writes=[gTb.k()])
                ob_ = 1
                if kind == 0:
                    for jc in range(2):
                        P.pe(lambda e, jc=jc, hs=hs: e.matmul(ps[ob_][hs, 0:127], lhsT=w2[:, jc, :], rhs=gT[:, jc, 0:127],
                                                              start=(jc == 0), stop=(jc == 1)),
                             reads=[w2b.k(), gTb.k()], psum=[ob_])
                    P.dve(lambda e, hs=hs, j=j: e.tensor_scalar(out=kccT[hs, j, 0:127], in0=ps[ob_][hs, 0:127],
                                                                scalar1=b2c[hs, :], scalar2=None, op0=ADD),
                          reads=[b2cb.k()], writes=[kccb.k(hk)], psum=[ob_])
                else:
                    for jc in range(2):
                        P.pe(lambda e, jc=jc: e.matmul(ps[ob_][0:127, 0:64], lhsT=gT[:, jc, 0:127], rhs=w2[:, jc, :],
                                                       start=(jc == 0), stop=(jc == 1)),
                             reads=[w2b.k(), gTb.k()], psum=[ob_])
                    P.dve(lambda e, hk=hk: e.tensor_tensor(out=vcc[0:127, hk, 0:64], in0=ps[ob_][0:127, 0:64],
                                                           in1=b2r[0:127, :], op=ADD),
                          reads=[b2rb.k()], writes=[vccb.k(hk)], psum=[ob_])
        A.release(mk3)
        A.release(mkc)
        wob = A.alloc("wo", 8 * D // 2)
        wo = A.bf(wob).rearrange("p (k n) -> p k n", k=8)
        P.dma('pool', 'wo', lambda e: e.dma_start(out=wo, in_=nsa_wo_d[a].rearrange("(k p) n -> p k n", p=128)),
              writes=[wob.k()])
        qring2 = alloc_q_slots()
        pts_ = [A.alloc("pT", 256) for _ in range(3)]
        ptr_ = Ring([0, 1, 2])
        oaccbs = [A.alloc("oacc", D) for _ in range(2)]
        obfb = A.alloc("obf", D // 2)
        obf = A.bf(obfb)
        oTb = A.alloc("oT", D // 2)
        oT = A.bf(oTb).rearrange("p (c n) -> p c n", c=8)
        pts2 = [A.alloc("post_t1", 512) for _ in range(2)]
        ptring = Ring([(A.f32(pts2[i]), pts2[i].k()) for i in range(2)])
        ecb = A.alloc("ecmp", 512)
        ecmp = A.f32(ecb).rearrange("p (g n) -> p g n", g=4)
        pbb = A.alloc("pb", 5 * 128 // 2)
        pb_ = A.bf(pbb).rearrange("p (g n) -> p g n", g=5)
        pcTb = A.alloc("pcT", 5 * 128 // 2)
        pcT = A.bf(pcTb).rearrange("p (g n) -> p g n", g=5)
        impb = A.alloc("imp", 32)
        imp = A.f32(impb)
        selbb = A.alloc("selb", 16)
        selb = A.bf(selbb)
        selTb = A.alloc("selT", 4 * 128 // 2)
        selT = A.bf(selTb).rearrange("p (h n) -> p h n", h=4)
        P.pool(lambda e: e.memset(selT, 0.0), writes=[selTb.k('z')] + [selTb.k(h_) for h_ in range(4)])
        brt = [A.alloc("brtmp", 256) for _ in range(2)]
        brring = Ring([0, 1])
        sbank = Ring([3, 4, 7])
        branches = ((ksT, ksTb, VS, VSb, 5), (kwT, kwTb, VW, VWb, 6))

        def chain_stages(qi, hk, qsel, qkeys, oaccb):
            j, half = hk // 2, hk % 2
            hs = slice(half * 64, half * 64 + 64)
            qh = qsel[half]
            oacc = A.f32(oaccb).rearrange("p (h d) -> p h d", h=16)
            cb_, tb_, ob_ = 0, 1, 2
            cv = ps[cb_][:, :].rearrange("p (g n) -> p g n", g=4)
            tv = psb(tb_)[:, 0:640].rearrange("p (g n) -> p g n", g=5)
            tv2 = psb(tb_)[:, 768:896]
            ocv = ps[ob_][:, 0:256].rearrange("p (g d) -> p g d", g=4)
            gsl0 = GT[:, qi, 12 * hk:12 * hk + 10:3]

            def stA():
                for g in range(4):
                    P.pe(lambda e, g=g: e.matmul(
                        cv[:, g, 0:127], lhsT=qh[hs, 4 * j + g, :], rhs=kccT[hs, j, 0:127],
                        start=(g == 0), stop=False, skip_group_check=True),
                        reads=qkeys + [kccb.k(hk)], psum=[cb_])
                    P.pe(lambda e, g=g: e.matmul(
                        cv[:, g, 0:127], lhsT=ident, rhs=cmask[:, 120 - 8 * qi:247 - 8 * qi],
                        start=False, stop=True, skip_group_check=True), reads=[kIDENT, CB2b.k()], psum=[cb_])

            def stB():
                P.act(lambda e: e.activation(out=ecmp[:, :, 0:127], in_=cv[:, :, 0:127], func=AF.Exp, scale=0.125),
                      writes=[ecb.k()], psum=[cb_])
                dn, dnk = small_ring.next()
                P.dve(lambda e: e.tensor_reduce(out=dn[:, 0:4], in_=ecmp[:, :, 0:127], axis=AX.X, op=ADD),
                      reads=[ecb.k()], writes=[dnk])
                P.dve(lambda e: e.tensor_scalar(out=dn[:, 0:4], in0=dn[:, 0:4], scalar1=1e-30, scalar2=None, op0=MAX),
                      reads=[dnk], writes=[dnk])
                P.dve(lambda e: e.reciprocal(out=dn[:, 4:8], in_=dn[:, 0:4]), reads=[dnk], writes=[dnk])
                P.dve(lambda e: e.tensor_tensor(
                    out=pb_[:, 0:4, 0:127], in0=ecmp[:, :, 0:127],
                    in1=dn[:, 4:8].unsqueeze(2).to_broadcast([128, 4, 127]), op=MUL),
                    reads=[ecb.k(), dnk], writes=[pbb.k(0)])
                P.dve(lambda e: e.tensor_reduce(out=pb_[:, 4, 0:127], in_=pb_[:, 0:4, 0:127].rearrange("p g n -> p n g"),
                                                axis=AX.X, op=ADD), reads=[pbb.k(0)], writes=[pbb.k(1)])

            def stC():
                for g in range(5):
                    P.pe(lambda e, g=g: e.transpose(out=tv[0:127, g, :], in_=pb_[:, g, 0:127], identity=ident),
                         reads=[pbb.k(0), pbb.k(1), kIDENT], psum=[tb_])
                P.dve(lambda e: e.tensor_copy(out=pcT[0:127], in_=tv[0:127]), writes=[pcTb.k()], psum=[tb_])

            def stD():
                for g in range(4):
                    P.pe(lambda e, g=g: e.matmul(ocv[:, g, :], lhsT=pcT[0:127, g, :], rhs=vcc[0:127, hk, 0:64],
                                                 start=(g == 0), stop=True, skip_group_check=True),
                         reads=[pcTb.k(), vccb.k(hk)], psum=[ob_])
                P.pe(lambda e: e.matmul(ps[ob_][:, 256:288], lhsT=pcT[0:127, 4, :], rhs=Mmat[0:127, :],
                                        start=False, stop=True, skip_group_check=True),
                     reads=[pcTb.k(), CB2b.k()], psum=[ob_])
                P.dve(lambda e: e.tensor_tensor(
                    out=oacc[:, 4 * hk:4 * hk + 4, :], in0=ocv,
                    in1=gsl0.unsqueeze(2).to_broadcast([128, 4, 64]), op=MUL),
                    reads=[GTb.k(qi)], writes=[oaccb.k(hk)], psum=[ob_])
                P.dve(lambda e: e.tensor_tensor(out=imp, in0=ps[ob_][:, 256:288], in1=Aadj[:, qi, :], op=MUL),
                      reads=[CFb.k()], writes=[impb.k()], psum=[ob_])
                P.dve(lambda e: e.tensor_tensor(out=imp, in0=imp, in1=Badj[:, qi, :], op=ADD),
                      reads=[impb.k(), CFb.k()], writes=[impb.k()])
                mx, mxk = small_ring.next()
                P.dve(lambda e: e.max(out=mx[:, 0:8], in_=imp), reads=[impb.k()], writes=[mxk])
                P.dve(lambda e: e.tensor_scalar(out=selb, in0=imp, scalar1=mx[:, 7:8], scalar2=1.0,
                                                op0=ALU.is_ge, op1=SUB), reads=[impb.k(), mxk], writes=[selbb.k()])

            def stE():
                P.pe(lambda e: e.transpose(out=tv2[0:32, :], in_=selb, identity=ident),
                     reads=[selbb.k(), kIDENT], psum=[tb_])
                P.dve(lambda e: e.tensor_copy(out=selT[0:32, hk, :], in_=tv2[0:32, :]),
                      writes=[selTb.k(hk)], psum=[tb_])

            return [stA, stB, stC, stD, stE]

        q_cur = load_q_tile(qring2, 0)
        for st in chain_stages(0, 0, q_cur[0], q_cur[1], oaccbs[0]):
            st()
        prev_fin = None
        for qi in range(NT):
            q_next = load_q_tile(qring2, qi + 1) if qi + 1 < NT else None
            qsel, qkeys = q_cur
            oaccb = oaccbs[qi % 2]
            oacc = A.f32(oaccb).rearrange("p (h d) -> p h d", h=16)
            jobs_all = []
            pending = []
            for hk in range(4):
                jobs = []
                for br in range(2):
                    kts = list(range(0, qi + 1)) if br == 0 else list(range(max(0, qi - 4), qi + 1))
                    for ki, kt in enumerate(kts):
                        jobs.append(dict(hk=hk, br=br, kt=kt, ki=ki, nk=len(kts)))
                base = len(jobs_all)
                if hk < 3:
                    nxt = chain_stages(qi, hk + 1, qsel, qkeys, oaccb)
                elif q_next is not None:
                    nxt = chain_stages(qi + 1, 0, q_next[0], q_next[1], oaccbs[(qi + 1) % 2])
                else:
                    nxt = []
                slots = len(jobs) - 2
                fracs = (0.0, 0.0, 0.45, 0.7, 0.9)
                for si, st in enumerate(nxt):
                    if slots >= 1:
                        pending.append((base + min(slots - 1, int(fracs[si] * slots)), st))
                    else:
                        pending.append((base - 1, st))
                jobs_all += jobs

            def emit_qk(jb, qi=qi, qsel=qsel, qkeys=qkeys):
                sb_ = sbank.next()
                jb['sb'] = sb_
                hk, br, kt = jb['hk'], jb['br'], jb['kt']
                j, half = hk // 2, hk % 2
                kT_, kTb_ = branches[br][0], branches[br][1]
                need_mask = (kt == qi) or (br == 0) or (kt == qi - 4)
                P.pe(lambda e: e.matmul(
                    ps[sb_][:, :].rearrange("p (g n) -> p g n", g=4),
                    lhsT=kT_[:, j, kt * 128:(kt + 1) * 128], rhs=qsel[half][:, 4 * j:4 * j + 4, :],
                    start=True, stop=(not need_mask)), reads=[kTb_.k(j, kt // 4)] + qkeys, psum=[sb_])
                if kt == qi:
                    P.pe(lambda e: e.matmul(ps[sb_][:, :], lhsT=ident, rhs=triD, start=False, stop=True),
                         reads=[kIDENT], psum=[sb_])
                elif br == 0:
                    P.pe(lambda e: e.matmul(
                        ps[sb_][:, :].rearrange("p (g n) -> p g n", g=4),
                        lhsT=Emat[:, kt * 128:(kt + 1) * 128],
                        rhs=selT[:, hk, :].unsqueeze(1).to_broadcast([128, 4, 128]),
                        start=False, stop=True), reads=[Eb.k(), selTb.k(hk), selTb.k('z')], psum=[sb_])
                elif kt == qi - 4:
                    P.pe(lambda e: e.matmul(ps[sb_][:, :], lhsT=ident, rhs=triW, start=False, stop=True),
                         reads=[kIDENT], psum=[sb_])

            def emit_rest(jb, qi=qi, oacc=oacc, oaccb=oaccb):
                sb_, hk, br, kt, ki, nk = jb['sb'], jb['hk'], jb['br'], jb['kt'], jb['ki'], jb['nk']
                Vv, Vb_, obank = branches[br][2], branches[br][3], branches[br][4]
                ov = ps[obank][:, 0:260].rearrange("p (g d) -> p g d", g=4)
                ptb = pts_[ptr_.next()]
                pt = A.bf(ptb)
                P.act(lambda e: e.activation(out=pt, in_=ps[sb_][:, :], func=AF.Exp, scale=0.125),
                      writes=[ptb.k()], psum=[sb_])
                for g in range(4):
                    P.pe(lambda e, g=g: e.matmul(
                        ov[:, g, :], lhsT=pt[:, g * 128:(g + 1) * 128], rhs=Vv[:, kt, hk, :],
                        start=(ki == 0 and g == 0), stop=(ki == nk - 1), skip_group_check=True),
                        reads=[ptb.k(), Vb_.k(kt), Vb_.k('ones')], psum=[obank])
                if ki != nk - 1:
                    return
                gsl = GT[:, qi, 12 * hk + 1 + br:12 * hk + 1 + br + 10:3]
                dn, dnk = small_ring.next()
                P.dve(lambda e: e.reciprocal(out=dn[:, 0:4], in_=ov[:, :, 64]), writes=[dnk], psum=[obank])
                P.dve(lambda e: e.tensor_tensor(out=dn[:, 4:8], in0=dn[:, 0:4], in1=gsl, op=MUL),
                      reads=[dnk, GTb.k(qi)], writes=[dnk])
                btb = brt[brring.next()]
                bt = A.f32(btb).rearrange("p (g d) -> p g d", g=4)
                P.dve(lambda e: e.tensor_tensor(
                    out=bt, in0=ov[:, :, 0:64], in1=dn[:, 4:8].unsqueeze(2).to_broadcast([128, 4, 64]), op=MUL),
                    reads=[dnk], writes=[btb.k()], psum=[obank])
                P.pool(lambda e: e.tensor_tensor(out=oacc[:, 4 * hk:4 * hk + 4, :],
                                                 in0=oacc[:, 4 * hk:4 * hk + 4, :], in1=bt, op=ADD),
                       reads=[btb.k(), oaccb.k(hk)], writes=[oaccb.k(hk)])

            for (pos, st) in pending:
                if pos == -1:
                    st()
            for jb in jobs_all[:2]:
                emit_qk(jb)
            for i, jb in enumerate(jobs_all):
                if i + 2 < len(jobs_all):
                    emit_qk(jobs_all[i + 2])
                emit_rest(jb)
                for (pos, st) in pending:
                    if pos == i:
                        st()
                if prev_fin is not None and i == min(3, len(jobs_all) - 1):
                    prev_fin()

            def fin(qi=qi, oaccb=oaccb):
                attn_out_and_residual(qi, A.f32(oaccb), [oaccb.k(h_) for h_ in range(4)], obf, obfb.k(), oT, oTb,
                                      wo, wob.k(), ptring, [0, 2], 1)
            prev_fin = fin
            q_cur = q_next
        prev_fin()
        A.release(mk)

    phase0()
    Xb = A.alloc("X", NT * D)
    X = A.f32(Xb).rearrange("p (t d) -> p t d", t=NT)
    MODb = A.alloc("modv", 3 * D)
    MOD = A.f32(MODb).rearrange("p (a d) -> p a d", a=3)
    CSb = A.alloc("cossin", 2 * S)
    COS = A.f32(CSb, S, 0)
    SIN = A.f32(CSb, S, S)
    SMb = A.alloc("small", 16 * 64)
    small_ring = Ring([(A.f32(SMb, 64, 64 * i), SMb.k(i)) for i in range(16)])
    JKb = A.alloc("junk", D // 2)
    junk = A.bf(JKb)
    stores = []
    for s in range(nseq):
        P.dma('sp', 'xload', lambda e, s=s: [
            e.dma_start(out=X[:, 4 * i:4 * i + 4, :], in_=x_d[s, 512 * i:512 * (i + 1), :].rearrange("(t p) d -> p t d", p=128))
            for i in range(4)], writes=[Xb.k(t) for t in range(NT)], ndma=4)
        rope_tables(s)
        for (l, kinds) in layers:
            if 'mix' in kinds:
                if l % 2 == 0:
                    nsa_sublayer(l, s)
                else:
                    swa_sublayer(l, s)
            if 'mlp' in kinds:
                mlp_sublayer(l, s)
        st = P.dma('sp', 'xstore', lambda e, s=s: [
            e.dma_start(out=out_d[s, 512 * i:512 * (i + 1), :].rearrange("(t p) d -> p t d", p=128), in_=X[:, 4 * i:4 * i + 4, :])
            for i in range(4)], reads=[Xb.k(t) for t in range(NT)], ndma=4)
        stores.append(st)
    with nc.allow_non_contiguous_dma("small transposed parameter loads"), \
            nc.allow_low_precision("bf16 matmul operands by design; sums accumulate in fp32 PSUM"):
        P.emit(nc, final_waits=stores)
    es.close()
    return nc, A.peak


_CACHE = {}


def make_in_maps(x, c, positions, ada_w, ada_b, norm_g, nsa_w_in, nsa_w_out, nsa_cmp_pe, nsa_phi_w1, nsa_phi_b1,
                 nsa_phi_w2, nsa_phi_b2, swa_w_in, swa_w_out, swa_sinks, mlp_w_up, mlp_w_down, ncores=8):
    f = lambda a: np.ascontiguousarray(np.asarray(a, dtype=np.float32))
    nsa_fm, nsa_tm, swa_fm, swa_tm = permute_weights(f(nsa_w_in), f(swa_w_in))
    hc = host_constants()
    shared = {
        "ada_w": f(ada_w), "ada_b": f(ada_b), "norm_g": f(norm_g),
        "nsa_fm": nsa_fm, "nsa_tm": nsa_tm, "nsa_wo": f(nsa_w_out), "cmp_pe": f(nsa_cmp_pe),
        "phi_w1": f(nsa_phi_w1), "phi_b1": f(nsa_phi_b1), "phi_w2": f(nsa_phi_w2), "phi_b2": f(nsa_phi_b2),
        "swa_fm": swa_fm, "swa_tm": swa_tm, "swa_wo": f(swa_w_out), "swa_sinks": f(swa_sinks),
        "mlp_w_up": f(mlp_w_up), "mlp_w_down": f(mlp_w_down),
        "cbf": hc['cbf'], "cE": hc['E'], "cbf2": hc['cbf2'], "cf32": hc['cf32'],
    }
    x = f(x)
    c = f(c)
    positions = np.ascontiguousarray(np.asarray(positions, dtype=np.int32))
    maps = []
    for i in range(ncores):
        m = dict(shared)
        m["x"] = np.ascontiguousarray(x[2 * i:2 * i + 2])
        m["c"] = np.ascontiguousarray(c[2 * i:2 * i + 2])
        m["pos"] = np.ascontiguousarray(positions[2 * i:2 * i + 2])
        maps.append(m)
    return maps


def kernel(**inputs):
    if 'nc' not in _CACHE:
        _CACHE['nc'] = build_program()[0]
    nc = _CACHE['nc']
    maps = make_in_maps(**inputs)
    res = run_bass_kernel_spmd(nc, maps, core_ids=list(range(8)))
    out = np.concatenate([np.asarray(r["out"]) for r in res.results], axis=0)
    return out.astype(np.float32)
```

```python
import contextlib
import numpy as np
import concourse.bass as bass
import concourse.mybir as mybir
from concourse.bass_utils import run_bass_kernel_spmd

F32 = mybir.dt.float32
BF16 = mybir.dt.bfloat16
I32 = mybir.dt.int32
AF = mybir.ActivationFunctionType
ALU = mybir.AluOpType
AX = mybir.AxisListType

ENG = ('pe', 'act', 'dve', 'pool', 'sp')
CENG = ('pe', 'act', 'dve', 'pool')
SEM_CH = 20000


class Op(object):
    __slots__ = ('eng', 'fn', 'waits', 'signal', 'sigval', 'pos', 'dma_sem', 'dma_val',
                 'ksnap', 'ndma')


class Prog(object):
    def __init__(self):
        self.ops = {e: [] for e in ENG}
        self.lastw = {}
        self.readers = {}
        self.known = {e: {} for e in ENG}
        self.dma_cnt = {}
        self.dma_sems = []

    def _src(self, op):
        if op.dma_sem is not None:
            return ('d', op.dma_sem), op.dma_val
        return op.eng, op.pos

    def alias(self, new_key, old_keys):
        rd = {}
        for ok in old_keys:
            cands = []
            w = self.lastw.get(ok)
            if w is not None:
                cands.append(w)
            r = self.readers.get(ok)
            if r:
                cands.extend(r.values())
            for op in cands:
                src, val = self._src(op)
                if src not in rd or self._src(rd[src])[1] < val:
                    rd[src] = op
        if rd:
            self.readers[new_key] = rd

    def add(self, eng, fn, reads=(), writes=(), psum=(), dma_sem=None, ndma=1):
        op = Op()
        op.eng = eng
        op.fn = fn
        op.signal = False
        op.sigval = None
        op.dma_sem = dma_sem
        op.ndma = ndma
        op.pos = len(self.ops[eng]) + 1
        if dma_sem is not None:
            if dma_sem not in self.dma_cnt:
                self.dma_cnt[dma_sem] = 0
                self.dma_sems.append(dma_sem)
            self.dma_cnt[dma_sem] += ndma
            op.dma_val = self.dma_cnt[dma_sem] * 16
        else:
            op.dma_val = None
        raw = []
        other = []
        for k in reads:
            w = self.lastw.get(k)
            if w is not None:
                raw.append(w)
        wkeys = list(writes) + [('PS', b) for b in psum]
        for k in wkeys:
            w = self.lastw.get(k)
            if w is not None:
                other.append(w)
            rd = self.readers.get(k)
            if rd:
                other.extend(rd.values())
        known = self.known[eng]
        waits = {}
        is_dma = dma_sem is not None

        def need(d, is_raw):
            if d.dma_sem is None and d.eng == eng and not is_dma and not is_raw:
                return
            src, val = self._src(d)
            if known.get(src, 0) >= val:
                return
            if waits.get(src, (0, None))[0] < val:
                waits[src] = (val, d)

        for d in raw:
            need(d, True)
        for d in other:
            need(d, False)
        op.waits = []
        for src, (val, d) in waits.items():
            known[src] = val
            d.signal = True
            op.waits.append(d)
            if d.ksnap is not None:
                for s2, v2 in d.ksnap:
                    if known.get(s2, 0) < v2:
                        known[s2] = v2
        if dma_sem is None:
            op.ksnap = tuple((e, known.get(e, 0)) for e in CENG if known.get(e, 0))
        else:
            op.ksnap = None
        for k in reads:
            rd = self.readers.get(k)
            if rd is None:
                rd = self.readers[k] = {}
            rd[self._src(op)[0]] = op
        for k in wkeys:
            self.lastw[k] = op
            self.readers[k] = {}
        self.ops[eng].append(op)
        return op

    def pe(self, fn, reads=(), writes=(), psum=()):
        return self.add('pe', fn, reads, writes, psum)

    def act(self, fn, reads=(), writes=(), psum=()):
        return self.add('act', fn, reads, writes, psum)

    def dve(self, fn, reads=(), writes=(), psum=()):
        return self.add('dve', fn, reads, writes, psum)

    def pool(self, fn, reads=(), writes=(), psum=()):
        return self.add('pool', fn, reads, writes, psum)

    def dma(self, q, sem, fn, reads=(), writes=(), ndma=1):
        return self.add(q, fn, reads, writes, (), dma_sem=sem, ndma=ndma)

    def emit(self, nc, final_waits=()):
        nsig = {}
        for e in ENG:
            c = 0
            for op in self.ops[e]:
                if op.dma_sem is None and op.signal:
                    c += 1
                    op.sigval = c
            nsig[e] = c
        with contextlib.ExitStack() as es:
            csem = {}
            for e in CENG:
                n = (nsig[e] + SEM_CH - 1) // SEM_CH
                csem[e] = [es.enter_context(nc.semaphore("s_%s_%d" % (e, i)))
                           for i in range(max(n, 1))]
            dsem = {}
            for i, s in enumerate(self.dma_sems):
                dsem[s] = es.enter_context(nc.semaphore("d%d" % i))
            block = es.enter_context(nc.Block())

            def semval(d):
                if d.dma_sem is not None:
                    return dsem[d.dma_sem], d.dma_val
                i = (d.sigval - 1) // SEM_CH
                return csem[d.eng][i], (d.sigval - 1) % SEM_CH + 1

            def run(e, eobj):
                for op in self.ops[e]:
                    for d in op.waits:
                        s, v = semval(d)
                        eobj.wait_ge(s, v)
                    r = op.fn(eobj)
                    if op.dma_sem is not None:
                        if not isinstance(r, (list, tuple)):
                            r = [r]
                        assert len(r) == op.ndma, (len(r), op.ndma)
                        for ins in r:
                            ins.then_inc(dsem[op.dma_sem], 16)
                    elif op.signal:
                        if isinstance(r, (list, tuple)):
                            r = r[-1]
                        s, v = semval(op)
                        r.then_inc(s, 1)
                if e == 'sp':
                    for d in final_waits:
                        s, v = semval(d)
                        eobj.wait_ge(s, v)

            @block.tensor
            def _(t):
                run('pe', t)

            @block.scalar
            def _(t):
                run('act', t)

            @block.vector
            def _(t):
                run('dve', t)

            @block.gpsimd
            def _(t):
                run('pool', t)

            @block.sync
            def _(t):
                run('sp', t)


class Buf(object):
    def __init__(self, P, name, start, words, ghosts):
        self.P = P
        self.name = name
        self.start = start
        self.words = words
        self.ghosts = ghosts
        self.keys = []
        self.keyset = set()

    def k(self, *idx):
        key = (self.name,) + idx
        if key not in self.keyset:
            self.keyset.add(key)
            self.keys.append(key)
            if self.ghosts:
                self.P.alias(key, self.ghosts)
        return key


class Arena(object):
    def __init__(self, P, tensor, words):
        self.P = P
        self.t = tensor
        self.words = words
        self.top = 0
        self.live = []
        self.ghosts = []
        self.n = 0
        self.peak = 0

    def alloc(self, name, words):
        words = (words + 3) // 4 * 4
        s, e = self.top, self.top + words
        assert e <= self.words, "SBUF arena overflow: %s needs %d, top %d of %d" % (
            name, words, self.top, self.words)
        self.top = e
        self.peak = max(self.peak, e)
        gk = []
        for (gs, ge, keys) in self.ghosts:
            if gs < e and ge > s:
                gk.extend(keys)
        self.n += 1
        b = Buf(self.P, "%s#%d" % (name, self.n), s, words, gk)
        self.live.append(b)
        return b

    def mark(self):
        return (self.top, len(self.live))

    def release(self, mark):
        top, nlive = mark
        for b in self.live[nlive:]:
            self.ghosts = [g for g in self.ghosts if not (g[0] >= b.start and g[1] <= b.start + b.words)]
            self.ghosts.append((b.start, b.start + b.words, list(b.keys)))
        del self.live[nlive:]
        self.top = top

    def f32(self, b, n=None, off=0):
        n = b.words - off if n is None else n
        return self.t[:, b.start + off:b.start + off + n]

    def bf(self, b, n=None, off=0):
        n = 2 * b.words - off if n is None else n
        assert off % 2 == 0
        w0 = b.start + off // 2
        return self.t[:, w0:w0 + (n + 1) // 2].bitcast(BF16)[:, 0:n]

    def i32(self, b, n=None, off=0):
        n = b.words - off if n is None else n
        return self.t[:, b.start + off:b.start + off + n].bitcast(I32)


class Ring(object):
    def __init__(self, items):
        self.items = items
        self.i = 0

    def next(self):
        it = self.items[self.i % len(self.items)]
        self.i += 1
        return it


D = 1024
S = 2048
NT = 16
HID = 4096
DEPTH = 4
BIG = 30000.0
BIGV = 1.0e9
EPS = 1e-6
ARENA_WORDS = 53000
PI = float(np.pi)


def host_constants():
    c = {}
    ident = np.eye(128, dtype=np.float32)
    rot = np.zeros((128, 128), np.float32)
    for m in range(128):
        if (m % 64) < 32:
            rot[m + 32, m] = -1.0
        else:
            rot[m - 32, m] = 1.0
    k = np.arange(128)[:, None]
    q = np.arange(128)[None, :]
    triD = np.where(k <= q, 0.0, -BIG).astype(np.float32)
    triW = np.where(k > q, 0.0, -BIG).astype(np.float32)
    c['cbf'] = np.concatenate([ident, rot, np.tile(triD, (1, 4)), np.tile(triW, (1, 4))], axis=1)
    E = np.zeros((128, 2048), np.float32)
    for j in range(32):
        E[j, j * 64:(j + 1) * 64] = BIG
    c['E'] = E
    cs = np.arange(127)[:, None] * 16
    ss = np.arange(32)[None, :] * 64
    ov = np.clip(np.minimum(cs + 32, ss + 64) - np.maximum(cs, ss), 0, None)
    M = np.zeros((128, 32), np.float32)
    M[:127] = ov / 16.0
    r = np.arange(128)[:, None]
    m = np.arange(248)[None, :]
    cm = np.where((m - 120) <= np.floor((r - 31) / 16.0), 0.0, -BIG).astype(np.float32)
    c['cbf2'] = np.concatenate([M, cm], axis=1)
    A = np.zeros((128, 16, 32), np.float32)
    B = np.zeros((128, 16, 32), np.float32)
    for qi in range(16):
        t = 128 * qi + np.arange(128)[:, None]
        cur = t // 64
        j = np.arange(32)[None, :]
        forced = (j == 0) | (j == cur) | (j == cur - 1)
        causal = j <= cur
        A[:, qi, :] = (causal & ~forced).astype(np.float32)
        B[:, qi, :] = np.where(forced, BIGV, np.where(causal, 0.0, -BIGV))
    inv = (1.0 / (10000.0 ** (np.arange(0, 64, 2, dtype=np.float32) / 64))).astype(np.float32)
    invcol = np.tile(inv, 4).reshape(128, 1)
    c['cf32'] = np.concatenate([A.reshape(128, 512), B.reshape(128, 512), invcol,
                                np.zeros((128, 3), np.float32)], axis=1).astype(np.float32)
    return c


def permute_weights(nsa_w_in, swa_w_in):
    cols = []
    for t in range(8):
        if t < 4:
            lo, hi = t, t + 4
        else:
            lo, hi = 8 + (t - 4), 12 + (t - 4)
        cols += list(range(lo * 64, lo * 64 + 64)) + list(range(hi * 64, hi * 64 + 64))
    cols += list(range(1024, 1280))
    cols += list(range(1280, 1536))
    cols += list(range(1536, 1792))
    cols += list(range(2048, 2304))
    nsa_fm = np.ascontiguousarray(nsa_w_in[:, :, cols])
    tm = list(range(1792, 2048)) + list(range(2304, 2560)) + list(range(2560, 2608))
    nsa_tm = np.ascontiguousarray(nsa_w_in[:, :, tm])
    cols = []
    for t in range(8):
        cols += list(range(t * 64, t * 64 + 64)) + list(range((t + 8) * 64, (t + 8) * 64 + 64))
    cols += list(range(1024, 1152))
    swa_fm = np.ascontiguousarray(swa_w_in[:, :, cols])
    swa_tm = np.ascontiguousarray(swa_w_in[:, :, 1152:1280])
    return nsa_fm, nsa_tm, swa_fm, swa_tm


def build_program(layers=None, nseq=2, debug=None):
    if layers is None:
        layers = [(l, ('mix', 'mlp')) for l in range(DEPTH)]
    nc = bass.Bass("TRN2", target_bir_lowering=False)
    es = contextlib.ExitStack()

    def din(name, shape, dt=F32):
        return nc.dram_tensor(name, list(shape), dt, kind="ExternalInput").ap()

    x_d = din("x", [2, S, D])
    c_d = din("c", [2, D])
    pos_d = din("pos", [2, S], I32)
    ada_w_d = din("ada_w", [4, D, 6 * D])
    ada_b_d = din("ada_b", [4, 6 * D])
    norm_g_d = din("norm_g", [4, 4, D])
    nsa_fm_d = din("nsa_fm", [2, D, 2048])
    nsa_tm_d = din("nsa_tm", [2, D, 560])
    nsa_wo_d = din("nsa_wo", [2, D, D])
    pe_d = din("cmp_pe", [2, 2, 32, 64])
    w1_d = din("phi_w1", [2, 2, 2048, 256])
    b1_d = din("phi_b1", [2, 2, 256])
    w2_d = din("phi_w2", [2, 2, 256, 64])
    b2_d = din("phi_b2", [2, 2, 64])
    swa_fm_d = din("swa_fm", [2, D, 1152])
    swa_tm_d = din("swa_tm", [2, D, 128])
    swa_wo_d = din("swa_wo", [2, D, D])
    sinks_d = din("swa_sinks", [2, 16])
    wup_d = din("mlp_w_up", [4, D, HID])
    wdn_d = din("mlp_w_down", [4, HID, D])
    cbf_d = din("cbf", [128, 1280])
    E_d = din("cE", [128, 2048])
    cbf2_d = din("cbf2", [128, 280])
    cf32_d = din("cf32", [128, 1028])
    out_d = nc.dram_tensor("out", [2, S, D], F32, kind="ExternalOutput").ap()
    modv_d = nc.dram_tensor("modv", [4, 2, 6, D], F32).ap()
    q_d = nc.dram_tensor("qscr", [128, 8, S], BF16).ap()

    arena_t = es.enter_context(nc.sbuf_tensor("arena", [128, ARENA_WORDS], F32))
    ps = [es.enter_context(nc.psum_tensor("ps%d" % i, [128, 512], F32)) for i in range(8)]
    P = Prog()
    A = Arena(P, arena_t, ARENA_WORDS)
    MUL, ADD, SUB, MAX = ALU.mult, ALU.add, ALU.subtract, ALU.max

    def psb(b):
        return ps[b][:, :].bitcast(BF16)

    CBb = A.alloc("cbf", 640)
    cbf = A.bf(CBb)
    ident = cbf[:, 0:128]
    rot = cbf[:, 128:256]
    triD = cbf[:, 256:768]
    triW = cbf[:, 768:1280]
    Eb = A.alloc("E", 1024)
    Emat = A.bf(Eb)
    CB2b = A.alloc("cbf2", 140)
    cbf2 = A.bf(CB2b)
    Mmat = cbf2[:, 0:32]
    cmask = cbf2[:, 32:280]
    CFb = A.alloc("cf32", 1028)
    cf32 = A.f32(CFb)
    Aadj = cf32[:, 0:512].rearrange("p (q j) -> p q j", q=16)
    Badj = cf32[:, 512:1024].rearrange("p (q j) -> p q j", q=16)
    invc = cf32[:, 1024:1025]
    NEGPIb = A.alloc("negpi", 4)
    negpi = A.f32(NEGPIb, 1, 0)
    epsc = A.f32(NEGPIb, 1, 1)
    halfpi = A.f32(NEGPIb, 1, 2)

    P.dma('pool', 'c0', lambda e: e.dma_start(out=cbf, in_=cbf_d), writes=[CBb.k()])
    P.dma('pool', 'c1', lambda e: e.dma_start(out=Emat, in_=E_d), writes=[Eb.k()])
    P.dma('pool', 'c2', lambda e: e.dma_start(out=cbf2, in_=cbf2_d), writes=[CB2b.k()])
    P.dma('sp', 'c3', lambda e: e.dma_start(out=cf32, in_=cf32_d), writes=[CFb.k()])
    P.pool(lambda e: e.memset(negpi, -PI), writes=[NEGPIb.k(0)])
    P.pool(lambda e: e.memset(epsc, EPS), writes=[NEGPIb.k(1)])
    P.pool(lambda e: e.memset(halfpi, PI / 2), writes=[NEGPIb.k(2)])
    KC = [CBb.k(), Eb.k(), CB2b.k(), CFb.k(), NEGPIb.k(0), NEGPIb.k(1)]
    kIDENT = CBb.k()

    def phase0():
        mk = A.mark()
        cTb = A.alloc("cT", 16)
        cT = A.f32(cTb).rearrange("p (k b) -> p k b", k=8)
        condb = A.alloc("cond", 8)
        cond = A.bf(condb).rearrange("p (k b) -> p k b", k=8)
        tmpb = A.alloc("ctmp", 16)
        ctmp = A.f32(tmpb).rearrange("p (k b) -> p k b", k=8)
        P.dma('sp', 'p0a', lambda e: [e.dma_start(out=cT[:, :, b_], in_=c_d[b_].rearrange("(k p) -> p k", p=128))
                                      for b_ in range(2)], writes=[cTb.k()], ndma=2)
        P.act(lambda e: e.activation(out=ctmp, in_=cT, func=AF.Exp, scale=-1.0), reads=[cTb.k()], writes=[tmpb.k()])
        P.dve(lambda e: e.tensor_scalar(out=ctmp, in0=ctmp, scalar1=1.0, scalar2=None, op0=ADD),
              reads=[tmpb.k()], writes=[tmpb.k()])
        P.dve(lambda e: e.reciprocal(out=ctmp, in_=ctmp), reads=[tmpb.k()], writes=[tmpb.k()])
        P.dve(lambda e: e.tensor_tensor(out=cond, in0=ctmp, in1=cT, op=MUL), reads=[tmpb.k(), cTb.k()],
              writes=[condb.k()])
        NQ = 1536
        wbufs = [A.alloc("adaw", 8 * NQ // 2) for _ in range(2)]
        wring = Ring(list(range(2)))
        modb = A.alloc("modsb", 6 * D)
        modsb = A.f32(modb)
        abb = A.alloc("adab", 6 * D)
        adab = A.f32(abb)
        gb = A.alloc("g4", 4 * D)
        g4 = A.f32(gb).rearrange("p (a d) -> p a d", a=4)
        effb = A.alloc("eff", 6 * D)
        eff = A.f32(effb).rearrange("p (a d) -> p a d", a=6)
        bank = Ring([0, 1, 2, 3])
        for l in range(DEPTH):
            P.dma('sp', 'p0b', lambda e, l=l: e.dma_start(out=adab[0:2, :], in_=ada_b_d[l:l + 1, :].to_broadcast([2, 6 * D])),
                  writes=[abb.k()])
            P.dma('sp', 'p0g', lambda e, l=l: e.dma_start(
                out=g4[0:2], in_=norm_g_d[l:l + 1].to_broadcast([2, 4, D])), writes=[gb.k()])
            for qt in range(4):
                wi = wring.next()
                wb = wbufs[wi]
                wv = A.bf(wb).rearrange("p (k n) -> p k n", k=8)
                P.dma('pool', ('adaw', wi), lambda e, l=l, qt=qt, wv=wv: e.dma_start(
                    out=wv, in_=ada_w_d[l, :, qt * NQ:(qt + 1) * NQ].rearrange("(k p) n -> p k n", p=128)),
                    writes=[wb.k()])
                for nchk in range(3):
                    b = bank.next()
                    for k in range(8):
                        P.pe(lambda e, b=b, k=k, wv=wv, nchk=nchk: e.matmul(
                            ps[b][0:2, :], lhsT=cond[:, k, :], rhs=wv[:, k, nchk * 512:(nchk + 1) * 512],
                            start=(k == 0), stop=(k == 7)), reads=[condb.k(), wb.k()], psum=[b])
                    c0 = qt * NQ + nchk * 512
                    P.dve(lambda e, b=b, c0=c0: e.tensor_tensor(out=modsb[0:2, c0:c0 + 512], in0=ps[b][0:2, :],
                                                                 in1=adab[0:2, c0:c0 + 512], op=ADD),
                          reads=[abb.k()], writes=[modb.k(c0)], psum=[b])
            allmod = [modb.k(c0) for c0 in range(0, 6 * D, 512)]
            plan = [(0, 1, 0), (2, 2, 1), (3, 4, 2), (5, 5, 3)]
            for (ei, mi, gi) in plan:
                P.dve(lambda e, ei=ei, mi=mi, gi=gi: e.scalar_tensor_tensor(
                    out=eff[0:2, ei, :], in0=modsb[0:2, mi * D:(mi + 1) * D], scalar=1.0, in1=g4[0:2, gi, :],
                    op0=ADD, op1=MUL), reads=allmod + [gb.k()], writes=[effb.k(ei)])
            for (ei, mi) in [(1, 0), (4, 3)]:
                P.dve(lambda e, ei=ei, mi=mi: e.tensor_copy(out=eff[0:2, ei, :], in_=modsb[0:2, mi * D:(mi + 1) * D]),
                      reads=allmod, writes=[effb.k(ei)])
            P.dma('sp', 'p0o', lambda e, l=l: e.dma_start(out=modv_d[l], in_=eff[0:2]),
                  reads=[effb.k(i) for i in range(6)], writes=[('modv_d', l)])
        A.release(mk)

    def load_mod(l, s, first):
        P.dma('sp', 'modl', lambda e: e.dma_start(
            out=MOD, in_=modv_d[l, s:s + 1, first:first + 3, :].to_broadcast([128, 3, D])),
            reads=[('modv_d', l)], writes=[MODb.k()])

    def rstd_from_ss(ss_ap, ss_key, n, out_ap, out_key):
        P.act(lambda e: e.activation(out=out_ap, in_=ss_ap, func=AF.Ln, scale=1.0 / D, bias=epsc),
              reads=[ss_key, NEGPIb.k(1)], writes=[out_key])
        P.act(lambda e: e.activation(out=out_ap, in_=out_ap, func=AF.Exp, scale=-0.5),
              reads=[out_key], writes=[out_key])

    def prenorm_tiles(tiles, hT, hTb, col0, bank_ring, tmpring, use_pool=True):
        ssap, sskey = small_ring.next()
        n = len(tiles)
        for i, t in enumerate(tiles):
            P.act(lambda e, t=t, i=i: e.activation(out=junk, in_=X[:, t, :], func=AF.Square,
                                                   accum_out=ssap[:, i:i + 1]),
                  reads=[Xb.k(t)], writes=[JKb.k(), sskey])
        rsap, rskey = small_ring.next()
        rstd_from_ss(ssap[:, 0:n], sskey, n, rsap[:, 0:n], rskey)
        for i, t in enumerate(tiles):
            (t1, t1k), (hb, hbk) = tmpring.next()
            P.dve(lambda e, t=t, i=i, t1=t1: e.scalar_tensor_tensor(
                out=t1, in0=X[:, t, :], scalar=rsap[:, i:i + 1], in1=MOD[:, 0, :], op0=MUL, op1=MUL),
                reads=[Xb.k(t), rskey, MODb.k()], writes=[t1k])
            (P.pool if use_pool else P.dve)(lambda e, t1=t1, hb=hb: e.tensor_tensor(out=hb, in0=t1, in1=MOD[:, 1, :], op=ADD),
                                            reads=[t1k, MODb.k()], writes=[hbk])
            b = bank_ring.next()
            pb = psb(b).rearrange("p (c n) -> p c n", c=8)
            for c in range(8):
                P.pe(lambda e, c=c, hb=hb, pb=pb: e.transpose(out=pb[:, c, :], in_=hb[:, c * 128:(c + 1) * 128],
                                                              identity=ident),
                     reads=[hbk, kIDENT], psum=[b])
            cc = col0 + i * 128
            P.act(lambda e, pb=pb, cc=cc: e.activation(out=hT[:, :, cc:cc + 128], in_=pb, func=AF.Copy),
                  writes=[hTb.k(cc // 128)], psum=[b])

    def post_residual(t, ybanks, eidx, tmpring, use_pool=True):
        ssap, sskey = small_ring.next()
        for h, b in enumerate(ybanks):
            P.act(lambda e, h=h, b=b: e.activation(out=junk[:, 0:512], in_=ps[b][:, :], func=AF.Square,
                                                   accum_out=ssap[:, h:h + 1]),
                  writes=[JKb.k(), sskey], psum=[b])
        P.dve(lambda e: e.tensor_tensor(out=ssap[:, 2:3], in0=ssap[:, 0:1], in1=ssap[:, 1:2], op=ADD),
              reads=[sskey], writes=[sskey])
        rsap, rskey = small_ring.next()
        rstd_from_ss(ssap[:, 2:3], sskey, 1, rsap[:, 0:1], rskey)
        for h, b in enumerate(ybanks):
            (t1, t1k) = tmpring.next()
            P.dve(lambda e, h=h, b=b, t1=t1: e.scalar_tensor_tensor(
                out=t1, in0=ps[b][:, :], scalar=rsap[:, 0:1], in1=MOD[:, eidx, h * 512:(h + 1) * 512],
                op0=MUL, op1=MUL), reads=[rskey, MODb.k()], writes=[t1k], psum=[b])
            (P.pool if use_pool else P.dve)(lambda e, h=h, t1=t1, t=t: e.tensor_tensor(
                out=X[:, t, h * 512:(h + 1) * 512], in0=X[:, t, h * 512:(h + 1) * 512], in1=t1, op=ADD),
                reads=[t1k, Xb.k(t)], writes=[Xb.k(t)])

    def mlp_sublayer(l, s):
        load_mod(l, s, 3)
        mk = A.mark()
        hTb = A.alloc("hTc", 8 * 512 // 2)
        hT = A.bf(hTb).rearrange("p (c n) -> p c n", c=8)
        aTb = A.alloc("aT", 32 * 512 // 2)
        aT = A.bf(aTb).rearrange("p (c n) -> p c n", c=32)
        NSL = 4
        ups = [A.alloc("wup", 8 * 256 // 2) for _ in range(NSL)]
        dns = [A.alloc("wdn", 2 * 1024 // 2) for _ in range(NSL)]
        pnring = alloc_pn_tmps()
        rs = [A.alloc("relu", 512) for _ in range(2)]
        rring = Ring([(A.f32(rs[i]), rs[i].k()) for i in range(2)])
        pts = [A.alloc("post_t1", 512) for _ in range(2)]
        ptring = Ring([(A.f32(pts[i]), pts[i].k()) for i in range(2)])
        upbank = Ring([0, 1, 2, 3])
        trbank = Ring([4, 5])
        items = []
        for tc in range(4):
            for sl in range(16):
                items.append(('u', tc, sl))
            for sl in range(16):
                items.append(('d', tc, sl))
        PF = 3
        loaded = [0]

        def issue_loads(upto):
            while loaded[0] < min(upto, len(items)):
                kind, tc, sl = items[loaded[0]]
                idx = loaded[0]
                loaded[0] += 1
                if kind == 'u':
                    ub = ups[(tc * 16 + sl) % NSL]
                    uv = A.bf(ub).rearrange("p (k n) -> p k n", k=8)
                    P.dma('pool', ('wup', (tc * 16 + sl) % NSL), lambda e, sl=sl, uv=uv: e.dma_start(
                        out=uv, in_=wup_d[l, :, sl * 256:(sl + 1) * 256].rearrange("(k p) n -> p k n", p=128)),
                        writes=[ub.k()])
                else:
                    db = dns[(tc * 16 + sl) % NSL]
                    dv = A.bf(db).rearrange("p (j n) -> p j n", j=2)
                    P.dma('pool', ('wdn', (tc * 16 + sl) % NSL), lambda e, sl=sl, dv=dv: e.dma_start(
                        out=dv, in_=wdn_d[l, sl * 256:(sl + 1) * 256, :].rearrange("(j p) n -> p j n", p=128)),
                        writes=[db.k()])

        pos = 0
        issue_loads(PF)
        prenorm_tiles([0, 1, 2, 3], hT, hTb, 0, trbank, pnring, use_pool=False)
        for tc in range(4):
            hkeys = [hTb.k(i) for i in range(4)]
            for sl in range(16):
                issue_loads(pos + 1 + PF)
                pos += 1
                ub = ups[(tc * 16 + sl) % NSL]
                uv = A.bf(ub).rearrange("p (k n) -> p k n", k=8)
                for j in range(2):
                    hc = sl * 2 + j
                    b = upbank.next()
                    for k in range(8):
                        P.pe(lambda e, b=b, k=k, uv=uv, j=j: e.matmul(
                            ps[b][:, :], lhsT=uv[:, k, j * 128:(j + 1) * 128], rhs=hT[:, k, :],
                            start=(k == 0), stop=(k == 7)), reads=hkeys + [ub.k()], psum=[b])
                    r, rk = rring.next()
                    if hc % 2 == 0:
                        P.act(lambda e, b=b, r=r: e.activation(out=r, in_=ps[b][:, :], func=AF.Relu),
                              writes=[rk], psum=[b])
                        P.dve(lambda e, r=r, hc=hc: e.tensor_tensor(out=aT[:, hc, :], in0=r, in1=r, op=MUL),
                              reads=[rk], writes=[aTb.k(hc)])
                    else:
                        P.dve(lambda e, b=b, r=r: e.tensor_scalar(out=r, in0=ps[b][:, :], scalar1=0.0, scalar2=None,
                                                                  op0=MAX), writes=[rk], psum=[b])
                        P.act(lambda e, r=r, hc=hc: e.activation(out=aT[:, hc, :], in_=r, func=AF.Square),
                              reads=[rk], writes=[aTb.k(hc)])
            if tc + 1 < 4:
                prenorm_tiles([4 * (tc + 1) + i for i in range(4)], hT, hTb, 0, trbank, pnring, use_pool=False)
            for sl in range(16):
                issue_loads(pos + 1 + PF)
                pos += 1
                db = dns[(tc * 16 + sl) % NSL]
                dv = A.bf(db).rearrange("p (j n) -> p j n", j=2)
                for tt in range(4):
                    for j in range(2):
                        hc = sl * 2 + j
                        for h in range(2):
                            b = tt * 2 + h
                            P.pe(lambda e, b=b, j=j, hc=hc, tt=tt, h=h, dv=dv: e.matmul(
                                ps[b][:, :], lhsT=aT[:, hc, tt * 128:(tt + 1) * 128],
                                rhs=dv[:, j, h * 512:(h + 1) * 512],
                                start=(hc == 0), stop=(hc == 31)), reads=[aTb.k(hc), db.k()], psum=[b])
            for tt in range(4):
                post_residual(4 * tc + tt, [tt * 2, tt * 2 + 1], 2, ptring, use_pool=False)
        A.release(mk)

    def rope_tables(s):
        mk = A.mark()
        pib = A.alloc("posi", S)
        posi = A.i32(pib)
        pfb = A.alloc("posf", S)
        posf = A.f32(pfb)
        C1 = 6.28125
        C2 = 2.0 * np.pi - 6.28125
        P.dma('sp', 'pos', lambda e: e.dma_start(out=posi, in_=pos_d[s:s + 1, :].to_broadcast([128, S])),
              writes=[pib.k()])
        P.dve(lambda e: e.tensor_copy(out=posf, in_=posi), reads=[pib.k()], writes=[pfb.k()])
        P.dve(lambda e: e.tensor_scalar(out=posf, in0=posf, scalar1=invc, scalar2=None, op0=MUL),
              reads=[pfb.k(), CFb.k()], writes=[pfb.k()])
        kS, kC = CSb.k(1), CSb.k(0)
        P.dve(lambda e: e.tensor_scalar(out=COS, in0=posf, scalar1=float(1.0 / (2 * np.pi)), scalar2=None, op0=MUL),
              reads=[pfb.k()], writes=[kC])
        P.dve(lambda e: e.tensor_copy(out=posi, in_=COS), reads=[kC, pib.k()], writes=[pib.k()])
        P.dve(lambda e: e.tensor_copy(out=COS, in_=posi), reads=[pib.k()], writes=[kC])
        P.dve(lambda e: e.scalar_tensor_tensor(out=SIN, in0=COS, scalar=-C1, in1=posf, op0=MUL, op1=ADD),
              reads=[kC, pfb.k()], writes=[kS])
        P.dve(lambda e: e.scalar_tensor_tensor(out=SIN, in0=COS, scalar=-C2, in1=SIN, op0=MUL, op1=ADD),
              reads=[kC, kS], writes=[kS])
        P.dve(lambda e: e.tensor_scalar(out=COS, in0=SIN, scalar1=PI, scalar2=-2 * PI, op0=ALU.is_gt, op1=MUL),
              reads=[kS], writes=[kC])
        P.dve(lambda e: e.tensor_tensor(out=SIN, in0=SIN, in1=COS, op=ADD), reads=[kS, kC], writes=[kS])
        P.dve(lambda e: e.tensor_scalar(out=COS, in0=SIN, scalar1=-PI, scalar2=2 * PI, op0=ALU.is_lt, op1=MUL),
              reads=[kS], writes=[kC])
        P.dve(lambda e: e.tensor_tensor(out=SIN, in0=SIN, in1=COS, op=ADD), reads=[kS, kC], writes=[kS])
        P.dve(lambda e: e.scalar_tensor_tensor(out=COS, in0=SIN, scalar=-1.0, in1=SIN, op0=MUL, op1=MAX), reads=[kS], writes=[kC])
        P.act(lambda e: e.activation(out=SIN, in_=SIN, func=AF.Sin), reads=[kS], writes=[kS])
        P.act(lambda e: e.activation(out=COS, in_=COS, func=AF.Sin, scale=-1.0, bias=halfpi),
              reads=[kC, NEGPIb.k(2)], writes=[kC])
        A.release(mk)

    def fm_project(hT, hkeys, wv, wkey, j, ntok0, tokbase, dest_fn, do_rope, banks, rtmp):
        b = banks.next()
        for k in range(8):
            P.pe(lambda e, b=b, k=k: e.matmul(ps[b][:, :], lhsT=wv[:, k, j * 128:(j + 1) * 128],
                                              rhs=hT[:, k, ntok0:ntok0 + 512], start=(k == 0), stop=(k == 7)),
                 reads=hkeys + [wkey], psum=[b])
        dst, dkey = dest_fn()
        if not do_rope:
            P.act(lambda e, b=b: e.activation(out=dst, in_=ps[b][:, :], func=AF.Copy), writes=[dkey], psum=[b])
            return
        (qraw, qrk), (t1, t1k), (t2, t2k) = rtmp.next()
        P.act(lambda e, b=b: e.activation(out=qraw, in_=ps[b][:, :], func=AF.Copy), writes=[qrk], psum=[b])
        b2 = banks.next()
        P.pe(lambda e, b2=b2: e.matmul(ps[b2][:, :], lhsT=rot, rhs=qraw, start=True, stop=True),
             reads=[qrk, kIDENT], psum=[b2])
        P.dve(lambda e, b=b: e.tensor_tensor(out=t1, in0=ps[b][:, :], in1=COS[:, tokbase:tokbase + 512], op=MUL),
              reads=[CSb.k(0)], writes=[t1k], psum=[b])
        P.dve(lambda e, b2=b2: e.tensor_tensor(out=t2, in0=ps[b2][:, :], in1=SIN[:, tokbase:tokbase + 512], op=MUL),
              reads=[CSb.k(1)], writes=[t2k], psum=[b2])
        P.pool(lambda e: e.tensor_tensor(out=dst, in0=t1, in1=t2, op=ADD), reads=[t1k, t2k], writes=[dkey])

    def alloc_rope_tmps(n=2):
        b_ = A.alloc("rt1", 512)
        c_ = A.alloc("rt2", 512)
        items = []
        for i in range(n):
            a = A.alloc("qraw", 256)
            items.append(((A.bf(a), a.k()), (A.f32(b_), b_.k()), (A.f32(c_), c_.k())))
        return Ring(items)

    def alloc_pn_tmps():
        t1 = A.alloc("pn_t1", D)
        hbs = [A.alloc("pn_hb", D // 2) for _ in range(2)]
        return Ring([((A.f32(t1), t1.k()), (A.bf(hbs[i]), hbs[i].k())) for i in range(2)])

    def attn_out_and_residual(t, oacc, oacck, obf, obfk, oT, oTb, wo, wokey, ptring, ybanks, trb):
        P.pool(lambda e: e.tensor_copy(out=obf, in_=oacc), reads=oacck, writes=[obfk])
        pb = psb(trb).rearrange("p (c n) -> p c n", c=8)
        for c in range(8):
            P.pe(lambda e, c=c: e.transpose(out=pb[:, c, :], in_=obf[:, c * 128:(c + 1) * 128], identity=ident),
                 reads=[obfk, kIDENT], psum=[trb])
        P.dve(lambda e: e.tensor_copy(out=oT, in_=pb), writes=[oTb.k()], psum=[trb])
        for h, b in enumerate(ybanks):
            for c in range(8):
                P.pe(lambda e, h=h, b=b, c=c: e.matmul(ps[b][:, :], lhsT=oT[:, c, :],
                                                       rhs=wo[:, c, h * 512:(h + 1) * 512],
                                                       start=(c == 0), stop=(c == 7)),
                     reads=[oTb.k(), wokey], psum=[b])
        post_residual(t, ybanks, 2, ptring)

    def alloc_q_slots():
        items = []
        for i in range(2):
            ba = A.alloc("qA", 8 * 128 // 2)
            bb = A.alloc("qB", 8 * 128 // 2)
            qa = A.bf(ba).rearrange("p (c n) -> p c n", c=8)
            qb_ = A.bf(bb).rearrange("p (c n) -> p c n", c=8)
            P.pool(lambda e, qa=qa: e.memset(qa[64:128], 0.0), writes=[ba.k('z')])
            P.pool(lambda e, qb_=qb_: e.memset(qb_[0:64], 0.0), writes=[bb.k('z')])
            items.append((i, ba, bb, qa, qb_))
        return Ring(items)

    def load_q_tile(ring, qi):
        i, ba, bb, qa, qb_ = ring.next()
        P.dma('sp', ('qt', i), lambda e: [
            e.dma_start(out=qa[0:64], in_=q_d[0:64, :, qi * 128:(qi + 1) * 128]),
            e.dma_start(out=qb_[64:128], in_=q_d[64:128, :, qi * 128:(qi + 1) * 128])],
            reads=[('q_d', ft, qi) for ft in range(8)], writes=[ba.k(), bb.k()], ndma=2)
        return (qa, qb_), [ba.k(), bb.k(), ba.k('z'), bb.k('z')]

    def run_jobs(jobs, emit_qk, emit_rest, hook=None):
        for jb in jobs[:2]:
            emit_qk(jb)
        for i, jb in enumerate(jobs):
            if i + 2 < len(jobs):
                emit_qk(jobs[i + 2])
            emit_rest(jb)
            if hook is not None and hook[1] is not None and i == min(hook[0], len(jobs) - 1):
                hook[1]()

    def swa_sublayer(l, s):
        a = l // 2
        load_mod(l, s, 0)
        mk = A.mark()
        kTb = A.alloc("kT", S // 2)
        kT = A.bf(kTb)
        Vb = A.alloc("V", NT * 2 * 65 // 2 + 2)
        V = A.bf(Vb, NT * 2 * 65).rearrange("p (t h d) -> p t h d", t=NT, h=2)
        esb = A.alloc("esink", 16)
        esink = A.f32(esb)
        P.dma('sp', 'sink', lambda e: e.dma_start(out=esink, in_=sinks_d[a:a + 1, :].to_broadcast([128, 16])),
              writes=[esb.k()])
        P.act(lambda e: e.activation(out=esink, in_=esink, func=AF.Exp), reads=[esb.k()], writes=[esb.k()])
        P.pool(lambda e: e.memset(V[:, :, :, 64:65], 1.0), writes=[Vb.k('ones')])
        mk2 = A.mark()
        hTbs = [A.alloc("hT", 8 * 512 // 2) for _ in range(2)]
        pnring = alloc_pn_tmps()
        rtmp = alloc_rope_tmps(2)
        NSLOT = 4
        wbig = A.alloc("wbig", NSLOT * 1024)
        qst = [A.alloc("qst", 256) for _ in range(3)]
        qring = Ring([0, 1, 2])
        trbank = Ring([6, 7])
        pjbank = Ring([0, 1, 2, 3, 4, 5])
        items = [(ch, si) for ch in range(4) for si in range(6)]
        loaded = [0]
        PF = 3

        def slab(idx):
            return (A.bf(wbig, 8 * 256, (idx % NSLOT) * 8 * 256).rearrange("p (k n) -> p k n", k=8),
                    wbig.k('s', idx % NSLOT))

        def issue_loads(upto):
            while loaded[0] < min(upto, len(items)):
                idx = loaded[0]
                loaded[0] += 1
                ch, si = items[idx]
                wv, wk = slab(idx)
                if si < 4:
                    src, ncols = swa_fm_d[a, :, si * 256:(si + 1) * 256], 256
                elif si == 4:
                    src, ncols = swa_fm_d[a, :, 1024:1152], 128
                else:
                    src, ncols = swa_tm_d[a], 128
                P.dma('pool', ('wfm', idx % NSLOT), lambda e, wv=wv, src=src, ncols=ncols: e.dma_start(
                    out=wv[:, :, 0:ncols], in_=src.rearrange("(k p) n -> p k n", p=128)), writes=[wk])

        pos = 0
        issue_loads(PF)

        def hview(ch):
            return A.bf(hTbs[ch % 2]).rearrange("p (c n) -> p c n", c=8), hTbs[ch % 2]

        prenorm_tiles([0, 1, 2, 3], hview(0)[0], hview(0)[1], 0, trbank, pnring)
        for ch in range(4):
            tiles = [4 * ch + i for i in range(4)]
            tok0 = ch * 512
            hT, hTb = hview(ch)
            hkeys = [hTb.k(i) for i in range(4)]
            for si in range(6):
                if si == 3 and ch + 1 < 4:
                    prenorm_tiles([4 * (ch + 1) + i for i in range(4)], hview(ch + 1)[0], hview(ch + 1)[1], 0,
                                  trbank, pnring)
                issue_loads(pos + 1 + PF)
                wv, wk = slab(pos)
                pos += 1
                if si < 4:
                    for j in range(2):
                        ft = si * 2 + j
                        qi_ = qring.next()
                        qb = qst[qi_]

                        def dest(qb=qb):
                            return A.bf(qb), qb.k()
                        fm_project(hT, hkeys, wv, wk, j, 0, tok0, dest, True, pjbank, rtmp)
                        P.dma('sp', ('qst', qi_), lambda e, qb=qb, ft=ft, tok0=tok0: e.dma_start(
                            out=q_d[:, ft, tok0:tok0 + 512], in_=A.bf(qb)), reads=[qb.k()],
                            writes=[('q_d', ft, tok0 // 128 + i) for i in range(4)])
                elif si == 4:
                    def dest(tok0=tok0):
                        return kT[:, tok0:tok0 + 512], kTb.k(tok0 // 512)
                    fm_project(hT, hkeys, wv, wk, 0, 0, tok0, dest, True, pjbank, rtmp)
                else:
                    for i, t in enumerate(tiles):
                        b = pjbank.next()
                        for k in range(8):
                            P.pe(lambda e, b=b, k=k, i=i, wv=wv, hT=hT: e.matmul(
                                ps[b][:, 0:128], lhsT=hT[:, k, i * 128:(i + 1) * 128], rhs=wv[:, k, 0:128],
                                start=(k == 0), stop=(k == 7)), reads=[hTb.k(i), wk], psum=[b])
                        P.dve(lambda e, b=b, t=t: e.tensor_copy(
                            out=V[:, t, :, 0:64], in_=ps[b][:, 0:128].rearrange("p (h d) -> p h d", h=2)),
                            writes=[Vb.k(t)], psum=[b])
        A.release(mk2)
        wob = A.alloc("wo", 8 * D // 2)
        wo = A.bf(wob).rearrange("p (k n) -> p k n", k=8)
        P.dma('pool', 'wo', lambda e: e.dma_start(out=wo, in_=swa_wo_d[a].rearrange("(k p) n -> p k n", p=128)),
              writes=[wob.k()])
        qring2 = alloc_q_slots()
        pts_ = [A.alloc("pT", 256) for _ in range(3)]
        ptr_ = Ring([0, 1, 2])
        oaccbs = [A.alloc("oacc", D) for _ in range(2)]
        prev_fin = None
        obfb = A.alloc("obf", D // 2)
        obf = A.bf(obfb)
        oTb = A.alloc("oT", D // 2)
        oT = A.bf(oTb).rearrange("p (c n) -> p c n", c=8)
        pts2 = [A.alloc("post_t1", 512) for _ in range(2)]
        ptring = Ring([(A.f32(pts2[i]), pts2[i].k()) for i in range(2)])
        sbank = Ring([3, 4, 7])
        obanks = {(0, 0): 0, (0, 1): 1, (1, 0): 0, (1, 1): 1}
        for qi in range(NT):
            qsel, qkeys = load_q_tile(qring2, qi)
            oaccb = oaccbs[qi % 2]
            oacc = A.f32(oaccb).rearrange("p (h d) -> p h d", h=16)
            kts = [kt for kt in (qi - 1, qi) if kt >= 0]
            jobs = []
            for kvh in range(2):
                for gh in range(2):
                    for ki, kt in enumerate(kts):
                        jobs.append(dict(kvh=kvh, gh=gh, ki=ki, kt=kt, nk=len(kts)))

            def emit_qk(jb, qi=qi, qsel=qsel, qkeys=qkeys):
                sb_ = sbank.next()
                jb['sb'] = sb_
                kt, gh, kvh = jb['kt'], jb['gh'], jb['kvh']
                P.pe(lambda e: e.matmul(
                    ps[sb_][:, :].rearrange("p (g n) -> p g n", g=4),
                    lhsT=kT[:, kt * 128:(kt + 1) * 128], rhs=qsel[kvh][:, 4 * gh:4 * gh + 4, :],
                    start=True, stop=False), reads=[kTb.k(kt // 4)] + qkeys, psum=[sb_])
                msk = triD if kt == qi else triW
                P.pe(lambda e: e.matmul(ps[sb_][:, :], lhsT=ident, rhs=msk, start=False, stop=True),
                     reads=[kIDENT], psum=[sb_])

            def emit_rest(jb, qi=qi, oacc=oacc, oaccb=oaccb):
                sb_, kt, gh, kvh, ki, nk = jb['sb'], jb['kt'], jb['gh'], jb['kvh'], jb['ki'], jb['nk']
                ob = obanks[(kvh, gh)]
                ov = ps[ob][:, 0:260].rearrange("p (g d) -> p g d", g=4)
                ptb = pts_[ptr_.next()]
                pt = A.bf(ptb)
                P.act(lambda e: e.activation(out=pt, in_=ps[sb_][:, :], func=AF.Exp, scale=0.125),
                      writes=[ptb.k()], psum=[sb_])
                for g in range(4):
                    P.pe(lambda e, g=g: e.matmul(
                        ov[:, g, :], lhsT=pt[:, g * 128:(g + 1) * 128], rhs=V[:, kt, kvh, :],
                        start=(ki == 0 and g == 0), stop=(ki == nk - 1), skip_group_check=True),
                        reads=[ptb.k(), Vb.k(kt), Vb.k('ones')], psum=[ob])
                if ki != nk - 1:
                    return
                h0 = kvh * 8 + gh * 4
                dn, dnk = small_ring.next()
                P.dve(lambda e: e.tensor_tensor(out=dn[:, 0:4], in0=ov[:, :, 64], in1=esink[:, h0:h0 + 4], op=ADD),
                      reads=[esb.k()], writes=[dnk], psum=[ob])
                P.dve(lambda e: e.reciprocal(out=dn[:, 4:8], in_=dn[:, 0:4]), reads=[dnk], writes=[dnk])
                P.dve(lambda e: e.tensor_tensor(
                    out=oacc[:, h0:h0 + 4, :], in0=ov[:, :, 0:64],
                    in1=dn[:, 4:8].unsqueeze(2).to_broadcast([128, 4, 64]), op=MUL),
                    reads=[dnk], writes=[oaccb.k(h0)], psum=[ob])

            def fin(qi=qi, oaccb=oaccb):
                attn_out_and_residual(qi, A.f32(oaccb), [oaccb.k(h0) for h0 in (0, 4, 8, 12)], obf, obfb.k(), oT, oTb,
                                      wo, wob.k(), ptring, [2, 5], 6)
            run_jobs(jobs, emit_qk, emit_rest, hook=(2, prev_fin))
            prev_fin = fin
        prev_fin()
        A.release(mk)

    def nsa_sublayer(l, s):
        a = l // 2
        load_mod(l, s, 0)
        mk = A.mark()
        ksTb = A.alloc("ksT", 2 * S // 2)
        ksT = A.bf(ksTb).rearrange("p (j n) -> p j n", j=2)
        kwTb = A.alloc("kwT", 2 * S // 2)
        kwT = A.bf(kwTb).rearrange("p (j n) -> p j n", j=2)
        VSb = A.alloc("VS", NT * 4 * 65 // 2 + 2)
        VS = A.bf(VSb, NT * 4 * 65).rearrange("p (t h d) -> p t h d", t=NT, h=4)
        VWb = A.alloc("VW", NT * 4 * 65 // 2 + 2)
        VW = A.bf(VWb, NT * 4 * 65).rearrange("p (t h d) -> p t h d", t=NT, h=4)
        GTb = A.alloc("gates", NT * 48)
        GT = A.f32(GTb).rearrange("p (t c) -> p t c", t=NT)
        kccb = A.alloc("kccT", 128)
        kccT = A.bf(kccb).rearrange("p (j n) -> p j n", j=2)
        vccb = A.alloc("vcc", 4 * 65 // 2 + 2)
        vcc = A.bf(vccb, 4 * 65).rearrange("p (h d) -> p h d", h=4)
        P.pool(lambda e: e.memset(VS[:, :, :, 64:65], 1.0), writes=[VSb.k('ones')])
        P.pool(lambda e: e.memset(VW[:, :, :, 64:65], 1.0), writes=[VWb.k('ones')])
        mkc = A.mark()
        kcTb = A.alloc("kcT", 2 * S // 2)
        kcT = A.bf(kcTb).rearrange("p (j n) -> p j n", j=2)
        vcTb = A.alloc("vcT", 2 * S // 2)
        vcT = A.bf(vcTb).rearrange("p (j n) -> p j n", j=2)
        mk2 = A.mark()
        hTb = A.alloc("hT", 8 * 512 // 2)
        hT = A.bf(hTb).rearrange("p (c n) -> p c n", c=8)
        pnring = alloc_pn_tmps()
        rtmp = alloc_rope_tmps(2)
        wbig = A.alloc("wbig", 4096)
        wring = Ring([0, 1])
        wtm = A.bf(wbig, 8 * 560, 0).rearrange("p (k n) -> p k n", k=8)
        wtmk = wbig.k('tm')
        qst = [A.alloc("qst", 256) for _ in range(3)]
        qring = Ring([0, 1, 2])
        gtmpb = A.alloc("gtmp", 48)
        gtmp = A.f32(gtmpb)
        trbank = Ring([6, 7])
        pjbank = Ring([0, 1, 2, 3, 4, 5])
        kdest = {8: (kcT, kcTb, 0), 9: (kcT, kcTb, 1), 10: (vcT, vcTb, 0), 11: (vcT, vcTb, 1),
                 12: (ksT, ksTb, 0), 13: (ksT, ksTb, 1), 14: (kwT, kwTb, 0), 15: (kwT, kwTb, 1)}
        for ch in range(4):
            tiles = [4 * ch + i for i in range(4)]
            tok0 = ch * 512
            prenorm_tiles(tiles, hT, hTb, 0, trbank, pnring)
            hkeys = [hTb.k(i) for i in range(4)]
            for sl in range(4):
                wi = wring.next()
                wv = A.bf(wbig, 8 * 512, wi * 8 * 512).rearrange("p (k n) -> p k n", k=8)
                wk = wbig.k('s', wi)
                P.dma('pool', ('wfm', wi), lambda e, sl=sl, wv=wv: e.dma_start(
                    out=wv, in_=nsa_fm_d[a, :, sl * 512:(sl + 1) * 512].rearrange("(k p) n -> p k n", p=128)),
                    writes=[wk, wtmk])
                for j in range(4):
                    ft = sl * 4 + j
                    if ft < 8:
                        qi_ = qring.next()
                        qb = qst[qi_]

                        def dest(qb=qb):
                            return A.bf(qb), qb.k()
                        fm_project(hT, hkeys, wv, wk, j, 0, tok0, dest, True, pjbank, rtmp)
                        P.dma('sp', ('qst', qi_), lambda e, qb=qb, ft=ft, tok0=tok0: e.dma_start(
                            out=q_d[:, ft, tok0:tok0 + 512], in_=A.bf(qb)), reads=[qb.k()],
                            writes=[('q_d', ft, tok0 // 128 + i) for i in range(4)])
                    else:
                        buf, bb, jj = kdest[ft]

                        def dest(buf=buf, bb=bb, jj=jj, tok0=tok0):
                            return buf[:, jj, tok0:tok0 + 512], bb.k(jj, tok0 // 512)
                        fm_project(hT, hkeys, wv, wk, j, 0, tok0, dest, ft not in (10, 11), pjbank, rtmp)
            P.dma('pool', 'wtm', lambda e: e.dma_start(out=wtm, in_=nsa_tm_d[a].rearrange("(k p) n -> p k n", p=128)),
                  writes=[wtmk, wbig.k('s', 0), wbig.k('s', 1)])
            for i, t in enumerate(tiles):
                b = pjbank.next()
                b2 = pjbank.next()
                for k in range(8):
                    P.pe(lambda e, b=b, k=k, i=i: e.matmul(ps[b][:, :], lhsT=hT[:, k, i * 128:(i + 1) * 128],
                                                           rhs=wtm[:, k, 0:512], start=(k == 0), stop=(k == 7)),
                         reads=[hTb.k(i), wtmk], psum=[b])
                for k in range(8):
                    P.pe(lambda e, b2=b2, k=k, i=i: e.matmul(ps[b2][:, 0:48], lhsT=hT[:, k, i * 128:(i + 1) * 128],
                                                             rhs=wtm[:, k, 512:560], start=(k == 0), stop=(k == 7)),
                         reads=[hTb.k(i), wtmk], psum=[b2])
                P.dve(lambda e, b=b, t=t: e.tensor_copy(
                    out=VS[:, t, :, 0:64], in_=ps[b][:, 0:256].rearrange("p (h d) -> p h d", h=4)),
                    writes=[VSb.k(t)], psum=[b])
                P.dve(lambda e, b=b, t=t: e.tensor_copy(
                    out=VW[:, t, :, 0:64], in_=ps[b][:, 256:512].rearrange("p (h d) -> p h d", h=4)),
                    writes=[VWb.k(t)], psum=[b])
                P.act(lambda e, b2=b2: e.activation(out=gtmp, in_=ps[b2][:, 0:48], func=AF.Exp, scale=-1.0),
                      writes=[gtmpb.k()], psum=[b2])
                P.pool(lambda e: e.tensor_scalar(out=gtmp, in0=gtmp, scalar1=1.0, scalar2=None, op0=ADD),
                       reads=[gtmpb.k()], writes=[gtmpb.k()])
                P.dve(lambda e, t=t: e.reciprocal(out=GT[:, t, :], in_=gtmp), reads=[gtmpb.k()], writes=[GTb.k(t)])
        A.release(mk2)
        mk3 = A.mark()
        w1b = A.alloc("w1", 32 * 256 // 2)
        w1 = A.bf(w1b).rearrange("p (l j) -> p l j", l=32)
        peb = A.alloc("peT", 16)
        peT = A.bf(peb)
        w2b = A.alloc("w2", 2 * 64 // 2)
        w2 = A.bf(w2b).rearrange("p (c d) -> p c d", c=2)
        b1b = A.alloc("b1T", 2)
        b1T = A.f32(b1b, 2)
        b2cb = A.alloc("b2c", 4)
        b2c = A.f32(b2cb, 1, 0)
        b2rb = A.alloc("b2r", 64)
        b2r = A.f32(b2rb)
        biasb = A.alloc("cbias", 2)
        cbias = A.f32(biasb, 2)
        gws = [A.alloc("gw%d" % i, 256) for i in range(4)]
        gx, gw_, ge, gr = [A.f32(g_).rearrange("p (c n) -> p c n", c=2) for g_ in gws]
        gTb = A.alloc("gT", 128)
        gT = A.bf(gTb).rearrange("p (c n) -> p c n", c=2)
        for kind in range(2):
            src = kcT if kind == 0 else vcT
            srcb = kcTb if kind == 0 else vcTb
            P.dma('pool', 'w1', lambda e, kind=kind: [
                e.dma_start(out=w1[0:64], in_=w1_d[a, kind].rearrange("(l d) j -> d l j", d=64)),
                e.dma_start(out=w1[64:128], in_=w1_d[a, kind].rearrange("(l d) j -> d l j", d=64))],
                writes=[w1b.k()], ndma=2)
            P.dma('pool', 'pe', lambda e, kind=kind: [
                e.dma_start(out=peT[0:64, :], in_=pe_d[a, kind].rearrange("l d -> d l")),
                e.dma_start(out=peT[64:128, :], in_=pe_d[a, kind].rearrange("l d -> d l"))],
                writes=[peb.k()], ndma=2)
            P.dma('pool', 'w2', lambda e, kind=kind: e.dma_start(
                out=w2, in_=w2_d[a, kind].rearrange("(c p) d -> p c d", p=128)), writes=[w2b.k()])
            P.dma('sp', 'b1', lambda e, kind=kind: e.dma_start(
                out=b1T, in_=b1_d[a, kind].rearrange("(c p) -> p c", p=128)), writes=[b1b.k()])
            if kind == 0:
                P.dma('sp', 'b2', lambda e: [
                    e.dma_start(out=b2c[0:64, :], in_=b2_d[a, 0].rearrange("(d o) -> d o", o=1)),
                    e.dma_start(out=b2c[64:128, :], in_=b2_d[a, 0].rearrange("(d o) -> d o", o=1))],
                    writes=[b2cb.k()], ndma=2)
            else:
                P.dma('sp', 'b2r', lambda e: e.dma_start(
                    out=b2r, in_=b2_d[a, 1:2, :].to_broadcast([128, 64])), writes=[b2rb.k()])
            for hk in range(4):
                j, half = hk // 2, hk % 2
                hs = slice(half * 64, half * 64 + 64)
                hb_ = 0
                hv = ps[hb_][:, 0:256].rearrange("p (c n) -> p c n", c=2)
                first = True
                for jc in range(2):
                    for li in range(32):
                        P.pe(lambda e, jc=jc, li=li, first=first, hs=hs, j=j, src=src: e.matmul(
                            hv[:, jc, 0:127], lhsT=w1[hs, li, jc * 128:(jc + 1) * 128],
                            rhs=src[hs, j, li:li + 16 * 126 + 1:16], start=first, stop=(li == 31),
                            skip_group_check=True),
                            reads=[w1b.k()] + [srcb.k(j, c_) for c_ in range(4)], psum=[hb_])
                        first = False
                        P.pe(lambda e, jc=jc, li=li, hs=hs: e.matmul(
                            hv[:, jc, 127:128], lhsT=w1[hs, li, jc * 128:(jc + 1) * 128], rhs=peT[hs, li:li + 1],
                            start=False, stop=(li == 31), skip_group_check=True),
                            reads=[w1b.k(), peb.k()], psum=[hb_])
                P.dve(lambda e, hv=hv: e.tensor_tensor(out=cbias, in0=hv[:, :, 127], in1=b1T, op=ADD),
                      reads=[b1b.k()], writes=[biasb.k()], psum=[hb_])
                for jc in range(2):
                    P.dve(lambda e, jc=jc, hv=hv: e.tensor_scalar(out=gx[:, jc, 0:127], in0=hv[:, jc, 0:127],
                                                                  scalar1=cbias[:, jc:jc + 1], scalar2=None, op0=ADD),
                          reads=[biasb.k()], writes=[gws[0].k(jc)], psum=[hb_])
                gxk = [gws[0].k(0), gws[0].k(1)]
                gx_, gwv, gev, grv = [g_[:, :, 0:127] for g_ in (gx, gw_, ge, gr)]
                P.pool(lambda e: e.tensor_tensor(out=gwv, in0=gx_, in1=gx_, op=MUL), reads=gxk, writes=[gws[1].k()])
                P.pool(lambda e: e.tensor_scalar(out=gwv, in0=gwv, scalar1=0.044715, scalar2=1.0, op0=MUL, op1=ADD),
                       reads=[gws[1].k()], writes=[gws[1].k()])
                P.pool(lambda e: e.tensor_tensor(out=gwv, in0=gwv, in1=gx_, op=MUL), reads=gxk + [gws[1].k()],
                       writes=[gws[1].k()])
                P.act(lambda e: e.activation(out=gev, in_=gwv, func=AF.Exp, scale=-1.5957691216057308),
                      reads=[gws[1].k()], writes=[gws[2].k()])
                P.pool(lambda e: e.tensor_scalar(out=gev, in0=gev, scalar1=1.0, scalar2=None, op0=ADD),
                       reads=[gws[2].k()], writes=[gws[2].k()])
                P.dve(lambda e: e.reciprocal(out=grv, in_=gev), reads=[gws[2].k()], writes=[gws[3].k()])
                P.pool(lambda e: e.tensor_tensor(out=gT[:, :, 0:127], in0=gx_, in1=grv, op=MUL),
                       reads=gxk + [gws[3].k()], writes=[gTb.k()])
                ob_ = 1
                if kind == 0:
                    for jc in range(2):
                        P.pe(lambda e, jc=jc, hs=hs: e.matmul(ps[ob_][hs, 0:127], lhsT=w2[:, jc, :], rhs=gT[:, jc, 0:127],
                                                              start=(jc == 0), stop=(jc == 1)),
                             reads=[w2b.k(), gTb.k()], psum=[ob_])
                    P.dve(lambda e, hs=hs, j=j: e.tensor_scalar(out=kccT[hs, j, 0:127], in0=ps[ob_][hs, 0:127],
                                                                scalar1=b2c[hs, :], scalar2=None, op0=ADD),
                          reads=[b2cb.k()], writes=[kccb.k(hk)], psum=[ob_])
                else:
                    for jc in range(2):
                        P.pe(lambda e, jc=jc: e.matmul(ps[ob_][0:127, 0:64], lhsT=gT[:, jc, 0:127], rhs=w2[:, jc, :],
                                                       start=(jc == 0), stop=(jc == 1)),
                             reads=[w2b.k(), gTb.k()], psum=[ob_])
                    P.dve(lambda e, hk=hk: e.tensor_tensor(out=vcc[0:127, hk, 0:64], in0=ps[ob_][0:127, 0:64],
                                                           in1=b2r[0:127, :], op=ADD),
                          reads=[b2rb.k()], writes=[vccb.k(hk)], psum=[ob_])
        A.release(mk3)
        A.release(mkc)
        wob = A.alloc("wo", 8 * D // 2)
        wo = A.bf(wob).rearrange("p (k n) -> p k n", k=8)
        P.dma('pool', 'wo', lambda e: e.dma_start(out=wo, in_=nsa_wo_d[a].rearrange("(k p) n -> p k n", p=128)),
              writes=[wob.k()])
        qring2 = alloc_q_slots()
        pts_ = [A.alloc("pT", 256) for _ in range(3)]
        ptr_ = Ring([0, 1, 2])
        oaccbs = [A.alloc("oacc", D) for _ in range(2)]
        obfb = A.alloc("obf", D // 2)
        obf = A.bf(obfb)
        oTb = A.alloc("oT", D // 2)
        oT = A.bf(oTb).rearrange("p (c n) -> p c n", c=8)
        pts2 = [A.alloc("post_t1", 512) for _ in range(2)]
        ptring = Ring([(A.f32(pts2[i]), pts2[i].k()) for i in range(2)])
        ecb = A.alloc("ecmp", 512)
        ecmp = A.f32(ecb).rearrange("p (g n) -> p g n", g=4)
        pbb = A.alloc("pb", 5 * 128 // 2)
        pb_ = A.bf(pbb).rearrange("p (g n) -> p g n", g=5)
        pcTb = A.alloc("pcT", 5 * 128 // 2)
        pcT = A.bf(pcTb).rearrange("p (g n) -> p g n", g=5)
        impb = A.alloc("imp", 32)
        imp = A.f32(impb)
        selbb = A.alloc("selb", 16)
        selb = A.bf(selbb)
        selTb = A.alloc("selT", 4 * 128 // 2)
        selT = A.bf(selTb).rearrange("p (h n) -> p h n", h=4)
        P.pool(lambda e: e.memset(selT, 0.0), writes=[selTb.k('z')] + [selTb.k(h_) for h_ in range(4)])
        brt = [A.alloc("brtmp", 256) for _ in range(2)]
        brring = Ring([0, 1])
        sbank = Ring([3, 4, 7])
        branches = ((ksT, ksTb, VS, VSb, 5), (kwT, kwTb, VW, VWb, 6))

        def chain_stages(qi, hk, qsel, qkeys, oaccb):
            j, half = hk // 2, hk % 2
            hs = slice(half * 64, half * 64 + 64)
            qh = qsel[half]
            oacc = A.f32(oaccb).rearrange("p (h d) -> p h d", h=16)
            cb_, tb_, ob_ = 0, 1, 2
            cv = ps[cb_][:, :].rearrange("p (g n) -> p g n", g=4)
            tv = psb(tb_)[:, 0:640].rearrange("p (g n) -> p g n", g=5)
            tv2 = psb(tb_)[:, 768:896]
            ocv = ps[ob_][:, 0:256].rearrange("p (g d) -> p g d", g=4)
            gsl0 = GT[:, qi, 12 * hk:12 * hk + 10:3]

            def stA():
                for g in range(4):
                    P.pe(lambda e, g=g: e.matmul(
                        cv[:, g, 0:127], lhsT=qh[hs, 4 * j + g, :], rhs=kccT[hs, j, 0:127],
                        start=(g == 0), stop=False, skip_group_check=True),
                        reads=qkeys + [kccb.k(hk)], psum=[cb_])
                    P.pe(lambda e, g=g: e.matmul(
                        cv[:, g, 0:127], lhsT=ident, rhs=cmask[:, 120 - 8 * qi:247 - 8 * qi],
                        start=False, stop=True, skip_group_check=True), reads=[kIDENT, CB2b.k()], psum=[cb_])

            def stB():
                P.act(lambda e: e.activation(out=ecmp[:, :, 0:127], in_=cv[:, :, 0:127], func=AF.Exp, scale=0.125),
                      writes=[ecb.k()], psum=[cb_])
                dn, dnk = small_ring.next()
                P.dve(lambda e: e.tensor_reduce(out=dn[:, 0:4], in_=ecmp[:, :, 0:127], axis=AX.X, op=ADD),
                      reads=[ecb.k()], writes=[dnk])
                P.dve(lambda e: e.tensor_scalar(out=dn[:, 0:4], in0=dn[:, 0:4], scalar1=1e-30, scalar2=None, op0=MAX),
                      reads=[dnk], writes=[dnk])
                P.dve(lambda e: e.reciprocal(out=dn[:, 4:8], in_=dn[:, 0:4]), reads=[dnk], writes=[dnk])
                P.dve(lambda e: e.tensor_tensor(
                    out=pb_[:, 0:4, 0:127], in0=ecmp[:, :, 0:127],
                    in1=dn[:, 4:8].unsqueeze(2).to_broadcast([128, 4, 127]), op=MUL),
                    reads=[ecb.k(), dnk], writes=[pbb.k(0)])
                P.dve(lambda e: e.tensor_reduce(out=pb_[:, 4, 0:127], in_=pb_[:, 0:4, 0:127].rearrange("p g n -> p n g"),
                                                axis=AX.X, op=ADD), reads=[pbb.k(0)], writes=[pbb.k(1)])

            def stC():
                for g in range(5):
                    P.pe(lambda e, g=g: e.transpose(out=tv[0:127, g, :], in_=pb_[:, g, 0:127], identity=ident),
                         reads=[pbb.k(0), pbb.k(1), kIDENT], psum=[tb_])
                P.dve(lambda e: e.tensor_copy(out=pcT[0:127], in_=tv[0:127]), writes=[pcTb.k()], psum=[tb_])

            def stD():
                for g in range(4):
                    P.pe(lambda e, g=g: e.matmul(ocv[:, g, :], lhsT=pcT[0:127, g, :], rhs=vcc[0:127, hk, 0:64],
                                                 start=(g == 0), stop=True, skip_group_check=True),
                         reads=[pcTb.k(), vccb.k(hk)], psum=[ob_])
                P.pe(lambda e: e.matmul(ps[ob_][:, 256:288], lhsT=pcT[0:127, 4, :], rhs=Mmat[0:127, :],
                                        start=False, stop=True, skip_group_check=True),
                     reads=[pcTb.k(), CB2b.k()], psum=[ob_])
                P.dve(lambda e: e.tensor_tensor(
                    out=oacc[:, 4 * hk:4 * hk + 4, :], in0=ocv,
                    in1=gsl0.unsqueeze(2).to_broadcast([128, 4, 64]), op=MUL),
                    reads=[GTb.k(qi)], writes=[oaccb.k(hk)], psum=[ob_])
                P.dve(lambda e: e.tensor_tensor(out=imp, in0=ps[ob_][:, 256:288], in1=Aadj[:, qi, :], op=MUL),
                      reads=[CFb.k()], writes=[impb.k()], psum=[ob_])
                P.dve(lambda e: e.tensor_tensor(out=imp, in0=imp, in1=Badj[:, qi, :], op=ADD),
                      reads=[impb.k(), CFb.k()], writes=[impb.k()])
                mx, mxk = small_ring.next()
                P.dve(lambda e: e.max(out=mx[:, 0:8], in_=imp), reads=[impb.k()], writes=[mxk])
                P.dve(lambda e: e.tensor_scalar(out=selb, in0=imp, scalar1=mx[:, 7:8], scalar2=1.0,
                                                op0=ALU.is_ge, op1=SUB), reads=[impb.k(), mxk], writes=[selbb.k()])

            def stE():
                P.pe(lambda e: e.transpose(out=tv2[0:32, :], in_=selb, identity=ident),
                     reads=[selbb.k(), kIDENT], psum=[tb_])
                P.dve(lambda e: e.tensor_copy(out=selT[0:32, hk, :], in_=tv2[0:32, :]),
                      writes=[selTb.k(hk)], psum=[tb_])

            return [stA, stB, stC, stD, stE]

        q_cur = load_q_tile(qring2, 0)
        for st in chain_stages(0, 0, q_cur[0], q_cur[1], oaccbs[0]):
            st()
        prev_fin = None
        for qi in range(NT):
            q_next = load_q_tile(qring2, qi + 1) if qi + 1 < NT else None
            qsel, qkeys = q_cur
            oaccb = oaccbs[qi % 2]
            oacc = A.f32(oaccb).rearrange("p (h d) -> p h d", h=16)
            jobs_all = []
            pending = []
            for hk in range(4):
                jobs = []
                for br in range(2):
                    kts = list(range(0, qi + 1)) if br == 0 else list(range(max(0, qi - 4), qi + 1))
                    for ki, kt in enumerate(kts):
                        jobs.append(dict(hk=hk, br=br, kt=kt, ki=ki, nk=len(kts)))
                base = len(jobs_all)
                if hk < 3:
                    nxt = chain_stages(qi, hk + 1, qsel, qkeys, oaccb)
                elif q_next is not None:
                    nxt = chain_stages(qi + 1, 0, q_next[0], q_next[1], oaccbs[(qi + 1) % 2])
                else:
                    nxt = []
                slots = len(jobs) - 2
                for si, st in enumerate(nxt):
                    if slots >= 1:
                        pending.append((base + min(slots - 1, si * slots // len(nxt)), st))
                    else:
                        pending.append((base - 1, st))
                jobs_all += jobs

            def emit_qk(jb, qi=qi, qsel=qsel, qkeys=qkeys):
                sb_ = sbank.next()
                jb['sb'] = sb_
                hk, br, kt = jb['hk'], jb['br'], jb['kt']
                j, half = hk // 2, hk % 2
                kT_, kTb_ = branches[br][0], branches[br][1]
                need_mask = (kt == qi) or (br == 0) or (kt == qi - 4)
                P.pe(lambda e: e.matmul(
                    ps[sb_][:, :].rearrange("p (g n) -> p g n", g=4),
                    lhsT=kT_[:, j, kt * 128:(kt + 1) * 128], rhs=qsel[half][:, 4 * j:4 * j + 4, :],
                    start=True, stop=(not need_mask)), reads=[kTb_.k(j, kt // 4)] + qkeys, psum=[sb_])
                if kt == qi:
                    P.pe(lambda e: e.matmul(ps[sb_][:, :], lhsT=ident, rhs=triD, start=False, stop=True),
                         reads=[kIDENT], psum=[sb_])
                elif br == 0:
                    P.pe(lambda e: e.matmul(
                        ps[sb_][:, :].rearrange("p (g n) -> p g n", g=4),
                        lhsT=Emat[:, kt * 128:(kt + 1) * 128],
                        rhs=selT[:, hk, :].unsqueeze(1).to_broadcast([128, 4, 128]),
                        start=False, stop=True), reads=[Eb.k(), selTb.k(hk), selTb.k('z')], psum=[sb_])
                elif kt == qi - 4:
                    P.pe(lambda e: e.matmul(ps[sb_][:, :], lhsT=ident, rhs=triW, start=False, stop=True),
                         reads=[kIDENT], psum=[sb_])

            def emit_rest(jb, qi=qi, oacc=oacc, oaccb=oaccb):
                sb_, hk, br, kt, ki, nk = jb['sb'], jb['hk'], jb['br'], jb['kt'], jb['ki'], jb['nk']
                Vv, Vb_, obank = branches[br][2], branches[br][3], branches[br][4]
                ov = ps[obank][:, 0:260].rearrange("p (g d) -> p g d", g=4)
                ptb = pts_[ptr_.next()]
                pt = A.bf(ptb)
                P.act(lambda e: e.activation(out=pt, in_=ps[sb_][:, :], func=AF.Exp, scale=0.125),
                      writes=[ptb.k()], psum=[sb_])
                for g in range(4):
                    P.pe(lambda e, g=g: e.matmul(
                        ov[:, g, :], lhsT=pt[:, g * 128:(g + 1) * 128], rhs=Vv[:, kt, hk, :],
                        start=(ki == 0 and g == 0), stop=(ki == nk - 1), skip_group_check=True),
                        reads=[ptb.k(), Vb_.k(kt), Vb_.k('ones')], psum=[obank])
                if ki != nk - 1:
                    return
                gsl = GT[:, qi, 12 * hk + 1 + br:12 * hk + 1 + br + 10:3]
                dn, dnk = small_ring.next()
                P.dve(lambda e: e.reciprocal(out=dn[:, 0:4], in_=ov[:, :, 64]), writes=[dnk], psum=[obank])
                P.dve(lambda e: e.tensor_tensor(out=dn[:, 4:8], in0=dn[:, 0:4], in1=gsl, op=MUL),
                      reads=[dnk, GTb.k(qi)], writes=[dnk])
                btb = brt[brring.next()]
                bt = A.f32(btb).rearrange("p (g d) -> p g d", g=4)
                P.dve(lambda e: e.tensor_tensor(
                    out=bt, in0=ov[:, :, 0:64], in1=dn[:, 4:8].unsqueeze(2).to_broadcast([128, 4, 64]), op=MUL),
                    reads=[dnk], writes=[btb.k()], psum=[obank])
                P.pool(lambda e: e.tensor_tensor(out=oacc[:, 4 * hk:4 * hk + 4, :],
                                                 in0=oacc[:, 4 * hk:4 * hk + 4, :], in1=bt, op=ADD),
                       reads=[btb.k(), oaccb.k(hk)], writes=[oaccb.k(hk)])

            for (pos, st) in pending:
                if pos == -1:
                    st()
            for jb in jobs_all[:2]:
                emit_qk(jb)
            for i, jb in enumerate(jobs_all):
                if i + 2 < len(jobs_all):
                    emit_qk(jobs_all[i + 2])
                emit_rest(jb)
                for (pos, st) in pending:
                    if pos == i:
                        st()
                if prev_fin is not None and i == min(3, len(jobs_all) - 1):
                    prev_fin()

            def fin(qi=qi, oaccb=oaccb):
                attn_out_and_residual(qi, A.f32(oaccb), [oaccb.k(h_) for h_ in range(4)], obf, obfb.k(), oT, oTb,
                                      wo, wob.k(), ptring, [0, 2], 1)
            prev_fin = fin
            q_cur = q_next
        prev_fin()
        A.release(mk)

    phase0()
    Xb = A.alloc("X", NT * D)
    X = A.f32(Xb).rearrange("p (t d) -> p t d", t=NT)
    MODb = A.alloc("modv", 3 * D)
    MOD = A.f32(MODb).rearrange("p (a d) -> p a d", a=3)
    CSb = A.alloc("cossin", 2 * S)
    COS = A.f32(CSb, S, 0)
    SIN = A.f32(CSb, S, S)
    SMb = A.alloc("small", 16 * 64)
    small_ring = Ring([(A.f32(SMb, 64, 64 * i), SMb.k(i)) for i in range(16)])
    JKb = A.alloc("junk", D // 2)
    junk = A.bf(JKb)
    stores = []
    for s in range(nseq):
        P.dma('sp', 'xload', lambda e, s=s: [
            e.dma_start(out=X[:, 4 * i:4 * i + 4, :], in_=x_d[s, 512 * i:512 * (i + 1), :].rearrange("(t p) d -> p t d", p=128))
            for i in range(4)], writes=[Xb.k(t) for t in range(NT)], ndma=4)
        rope_tables(s)
        for (l, kinds) in layers:
            if 'mix' in kinds:
                if l % 2 == 0:
                    nsa_sublayer(l, s)
                else:
                    swa_sublayer(l, s)
            if 'mlp' in kinds:
                mlp_sublayer(l, s)
        st = P.dma('sp', 'xstore', lambda e, s=s: [
            e.dma_start(out=out_d[s, 512 * i:512 * (i + 1), :].rearrange("(t p) d -> p t d", p=128), in_=X[:, 4 * i:4 * i + 4, :])
            for i in range(4)], reads=[Xb.k(t) for t in range(NT)], ndma=4)
        stores.append(st)
    with nc.allow_non_contiguous_dma("small transposed parameter loads"), \
            nc.allow_low_precision("bf16 matmul operands by design; sums accumulate in fp32 PSUM"):
        P.emit(nc, final_waits=stores)
    es.close()
    return nc, A.peak


_CACHE = {}


def make_in_maps(x, c, positions, ada_w, ada_b, norm_g, nsa_w_in, nsa_w_out, nsa_cmp_pe, nsa_phi_w1, nsa_phi_b1,
                 nsa_phi_w2, nsa_phi_b2, swa_w_in, swa_w_out, swa_sinks, mlp_w_up, mlp_w_down, ncores=8):
    f = lambda a: np.ascontiguousarray(np.asarray(a, dtype=np.float32))
    nsa_fm, nsa_tm, swa_fm, swa_tm = permute_weights(f(nsa_w_in), f(swa_w_in))
    hc = host_constants()
    shared = {
        "ada_w": f(ada_w), "ada_b": f(ada_b), "norm_g": f(norm_g),
        "nsa_fm": nsa_fm, "nsa_tm": nsa_tm, "nsa_wo": f(nsa_w_out), "cmp_pe": f(nsa_cmp_pe),
        "phi_w1": f(nsa_phi_w1), "phi_b1": f(nsa_phi_b1), "phi_w2": f(nsa_phi_w2), "phi_b2": f(nsa_phi_b2),
        "swa_fm": swa_fm, "swa_tm": swa_tm, "swa_wo": f(swa_w_out), "swa_sinks": f(swa_sinks),
        "mlp_w_up": f(mlp_w_up), "mlp_w_down": f(mlp_w_down),
        "cbf": hc['cbf'], "cE": hc['E'], "cbf2": hc['cbf2'], "cf32": hc['cf32'],
    }
    x = f(x)
    c = f(c)
    positions = np.ascontiguousarray(np.asarray(positions, dtype=np.int32))
    maps = []
    for i in range(ncores):
        m = dict(shared)
        m["x"] = np.ascontiguousarray(x[2 * i:2 * i + 2])
        m["c"] = np.ascontiguousarray(c[2 * i:2 * i + 2])
        m["pos"] = np.ascontiguousarray(positions[2 * i:2 * i + 2])
        maps.append(m)
    return maps


def kernel(**inputs):
    if 'nc' not in _CACHE:
        _CACHE['nc'] = build_program()[0]
    nc = _CACHE['nc']
    maps = make_in_maps(**inputs)
    res = run_bass_kernel_spmd(nc, maps, core_ids=list(range(8)))
    out = np.concatenate([np.asarray(r["out"]) for r in res.results], axis=0)
    return out.astype(np.float32)
```
